# Optimizing a Trainium2 kernel written in Bass

```python
import math
import jax
import jax.numpy as jnp
from jax import lax
import numpy as np

D_MODEL = 1024
BATCH = 1
SEQ = 16384
DEPTH = 1

ATTN_HEADS = 8
KV_HEADS = 2
HEAD_DIM = 64
Q_PER_KV = ATTN_HEADS // KV_HEADS
ATTN_WIDTH = ATTN_HEADS * HEAD_DIM
KV_WIDTH = KV_HEADS * HEAD_DIM
SSM_WIDTH = D_MODEL - ATTN_WIDTH
SSM_GROUP = 16
SSM_GROUPS = SSM_WIDTH // SSM_GROUP
SSM_STATE = 64
CMP_BLOCK = 32
CMP_STRIDE = 16
CMP_HIDDEN = 256
SEL_BLOCK = 64
SEL_TOPN = 16
WINDOW = 512
Q_BLOCK = 128
FORCE_SCORE = 1.0e4
ROPE_THETA = 10000.0
FFN_DIM = 2816
CONV_WIDTH = 3
NORM_EPS = 1e-6
IN_WIDTH = ATTN_WIDTH + 6 * KV_WIDTH + 3 * ATTN_HEADS + SSM_WIDTH

kernel_name = "hymba_nsa_s5_convffn_layer"


def rmsnorm(x, g):
    xf = x.astype(jnp.float32)
    y = xf * lax.rsqrt(jnp.mean(xf * xf, axis=-1, keepdims=True) + NORM_EPS)
    return (y * g.astype(jnp.float32)).astype(x.dtype)


def rope(x, pos):
    half = HEAD_DIM // 2
    inv = jnp.power(ROPE_THETA, -jnp.arange(half, dtype=jnp.float32) * 2.0 / HEAD_DIM)
    ang = pos.astype(jnp.float32)[:, None] * inv[None, :]
    cos = jnp.cos(ang)[None, :, None, :]
    sin = jnp.sin(ang)[None, :, None, :]
    xf = x.astype(jnp.float32)
    x1, x2 = xf[..., :half], xf[..., half:]
    return jnp.concatenate([x1 * cos - x2 * sin, x1 * sin + x2 * cos], axis=-1).astype(x.dtype)


def masked_softmax(s, mask):
    s = jnp.where(mask, s.astype(jnp.float32), -jnp.inf)
    m = jnp.max(s, axis=-1, keepdims=True)
    m = jnp.where(jnp.isfinite(m), m, 0.0)
    e = jnp.where(mask, jnp.exp(s - m), 0.0)
    return e / jnp.maximum(jnp.sum(e, axis=-1, keepdims=True), 1e-30)


def compress_blocks(kv, pe, w1, w2):
    b, l = kv.shape[0], kv.shape[1]
    n_cmp = (l - CMP_BLOCK) // CMP_STRIDE + 1
    idx = jnp.arange(n_cmp)[:, None] * CMP_STRIDE + jnp.arange(CMP_BLOCK)[None, :]
    blocks = kv[:, idx] + pe[None, None, :, None, :]
    flat = blocks.transpose(0, 1, 3, 2, 4).reshape(b, n_cmp, KV_HEADS, CMP_BLOCK * HEAD_DIM)
    out = jax.nn.gelu(flat @ w1) @ w2
    return out.transpose(0, 2, 1, 3)


def nsa_attention(q, k_c, v_c, k_s, v_s, k_w, v_w, gates, pe_k, pe_v, w_ck1, w_ck2, w_cv1, w_cv2):
    b, l = q.shape[0], q.shape[1]
    pos = jnp.arange(l)
    q = rope(q, pos)
    k_c = rope(k_c, pos)
    k_s = rope(k_s, pos)
    k_w = rope(k_w, pos)
    kc = compress_blocks(k_c, pe_k, w_ck1, w_ck2)
    vc = compress_blocks(v_c, pe_v, w_cv1, w_cv2)
    n_cmp = kc.shape[2]
    c_start = jnp.arange(n_cmp) * CMP_STRIDE
    c_end = c_start + CMP_BLOCK - 1
    n_sel = l // SEL_BLOCK
    top_n = min(SEL_TOPN, n_sel)
    s_start = jnp.arange(n_sel) * SEL_BLOCK
    overlap = jnp.clip(
        jnp.minimum(c_start[:, None] + CMP_BLOCK, s_start[None, :] + SEL_BLOCK)
        - jnp.maximum(c_start[:, None], s_start[None, :]), 0, None).astype(jnp.float32) / CMP_BLOCK
    ks_blk = k_s.reshape(b, n_sel, SEL_BLOCK, KV_HEADS, HEAD_DIM).transpose(0, 3, 1, 2, 4)
    vs_blk = v_s.reshape(b, n_sel, SEL_BLOCK, KV_HEADS, HEAD_DIM).transpose(0, 3, 1, 2, 4)
    pad = ((0, 0), (WINDOW, 0), (0, 0), (0, 0))
    kw_pad = jnp.pad(k_w, pad).transpose(0, 2, 1, 3)
    vw_pad = jnp.pad(v_w, pad).transpose(0, 2, 1, 3)
    n_blk = l // Q_BLOCK
    q_blk = q.reshape(b, n_blk, Q_BLOCK, KV_HEADS, Q_PER_KV, HEAD_DIM).transpose(1, 0, 3, 4, 2, 5)
    scale = HEAD_DIM ** -0.5
    b_idx = jnp.arange(b)[:, None, None, None]
    h_idx = jnp.arange(KV_HEADS)[None, :, None, None]
    sel_ids = jnp.arange(n_sel)
    win_off = jnp.arange(WINDOW + Q_BLOCK) - WINDOW
    sub = jnp.arange(SEL_BLOCK)

    def attend_block(args):
        qi, i = args
        t = i * Q_BLOCK + jnp.arange(Q_BLOCK)
        s_c = jnp.einsum('bhgqd,bhnd->bhgqn', qi, kc) * scale
        p_c = masked_softmax(s_c, c_end[None, :] <= t[:, None])
        o_c = jnp.einsum('bhgqn,bhnd->bhgqd', p_c.astype(vc.dtype), vc)
        imp = jnp.einsum('bhgqn,ns->bhqs', p_c, overlap)
        cur = (t // SEL_BLOCK)[:, None]
        forced = (sel_ids == 0) | (sel_ids == cur) | (sel_ids == cur - 1)
        score = jnp.where(sel_ids > cur, -jnp.inf, jnp.where(forced, FORCE_SCORE, imp))
        top_s, top_i = lax.top_k(score, top_n)
        k_g = ks_blk[b_idx, h_idx, top_i].reshape(b, KV_HEADS, Q_BLOCK, top_n * SEL_BLOCK, HEAD_DIM)
        v_g = vs_blk[b_idx, h_idx, top_i].reshape(b, KV_HEADS, Q_BLOCK, top_n * SEL_BLOCK, HEAD_DIM)
        key_pos = (top_i[..., None] * SEL_BLOCK + sub).reshape(b, KV_HEADS, Q_BLOCK, top_n * SEL_BLOCK)
        valid = jnp.repeat(jnp.isfinite(top_s), SEL_BLOCK, axis=-1) & (key_pos <= t[:, None])
        s_s = jnp.einsum('bhgqd,bhqkd->bhgqk', qi, k_g) * scale
        p_s = masked_softmax(s_s, valid[:, :, None])
        o_s = jnp.einsum('bhgqk,bhqkd->bhgqd', p_s.astype(v_g.dtype), v_g)
        k_wi = lax.dynamic_slice_in_dim(kw_pad, i * Q_BLOCK, WINDOW + Q_BLOCK, axis=2)
        v_wi = lax.dynamic_slice_in_dim(vw_pad, i * Q_BLOCK, WINDOW + Q_BLOCK, axis=2)
        w_pos = i * Q_BLOCK + win_off
        m_w = (w_pos[None, :] <= t[:, None]) & (w_pos[None, :] > t[:, None] - WINDOW) & (w_pos[None, :] >= 0)
        s_w = jnp.einsum('bhgqd,bhkd->bhgqk', qi, k_wi) * scale
        p_w = masked_softmax(s_w, m_w)
        o_w = jnp.einsum('bhgqk,bhkd->bhgqd', p_w.astype(v_wi.dtype), v_wi)
        return o_c, o_s, o_w

    o_c, o_s, o_w = lax.map(attend_block, (q_blk, jnp.arange(n_blk)))

    def unblock(o):
        return o.transpose(1, 0, 4, 2, 3, 5).reshape(b, l, ATTN_HEADS, HEAD_DIM)

    out = (gates[..., 0:1] * unblock(o_c) + gates[..., 1:2] * unblock(o_s)
           + gates[..., 2:3] * unblock(o_w))
    return out.reshape(b, l, ATTN_WIDTH)


def s5_mixer(u, lam_re, lam_im, log_step, b_re, b_im, c_re, c_im, d_skip, w_glu, b_glu):
    b, l, _ = u.shape
    uf = u.astype(jnp.float32).reshape(b, l, SSM_GROUPS, SSM_GROUP)
    step = jnp.exp(log_step.astype(jnp.float32))[:, None]
    lr = lam_re.astype(jnp.float32)
    li = lam_im.astype(jnp.float32)
    mag = jnp.exp(lr * step)
    a_re = mag * jnp.cos(li * step)
    a_im = mag * jnp.sin(li * step)
    den = lr * lr + li * li
    n_re = a_re - 1.0
    f_re = (n_re * lr + a_im * li) / den
    f_im = (a_im * lr - n_re * li) / den
    br = b_re.astype(jnp.float32)
    bi = b_im.astype(jnp.float32)
    bb_re = f_re[..., None] * br - f_im[..., None] * bi
    bb_im = f_re[..., None] * bi + f_im[..., None] * br
    bu_re = jnp.einsum('blgc,gpc->blgp', uf, bb_re)
    bu_im = jnp.einsum('blgc,gpc->blgp', uf, bb_im)
    a_re_t = jnp.broadcast_to(a_re, bu_re.shape)
    a_im_t = jnp.broadcast_to(a_im, bu_re.shape)

    def combine(e1, e2):
        a1r, a1i, b1r, b1i = e1
        a2r, a2i, b2r, b2i = e2
        return (a2r * a1r - a2i * a1i, a2r * a1i + a2i * a1r,
                a2r * b1r - a2i * b1i + b2r, a2r * b1i + a2i * b1r + b2i)

    _, _, x_re, x_im = lax.associative_scan(combine, (a_re_t, a_im_t, bu_re, bu_im), axis=1)
    y = (jnp.einsum('blgp,gcp->blgc', x_re, c_re.astype(jnp.float32))
         - jnp.einsum('blgp,gcp->blgc', x_im, c_im.astype(jnp.float32))
         + d_skip.astype(jnp.float32) * uf)
    y = jax.nn.gelu(y.reshape(b, l, SSM_WIDTH))
    y = y * jax.nn.sigmoid(y @ w_glu.astype(jnp.float32) + b_glu.astype(jnp.float32))
    return y.astype(u.dtype)


def causal_dwconv(u, w, bias):
    ch = u.shape[-1]
    y = lax.conv_general_dilated(u, w[:, None, :].astype(u.dtype), window_strides=(1,),
                                 padding=[(CONV_WIDTH - 1, 0)],
                                 dimension_numbers=('NWC', 'WIO', 'NWC'),
                                 feature_group_count=ch)
    return y + bias


def split_heads(t, n_heads):
    return t.reshape(t.shape[0], t.shape[1], n_heads, HEAD_DIM)


def setup_inputs(seed: int = 0) -> dict:
    key = jax.random.key(seed)
    ks = jax.random.split(key, 32)
    f32 = jnp.float32

    def nrm(k, shape, s):
        return jax.random.normal(k, shape, f32) * s

    D, F, NL = D_MODEL, FFN_DIM, DEPTH
    G, P, C = SSM_GROUPS, SSM_STATE, SSM_GROUP
    return {
        "x": nrm(ks[0], (BATCH, SEQ, D), 1.0),
        "c": nrm(ks[1], (BATCH, D), 1.0),
        "norm1_g": 1.0 + nrm(ks[2], (NL, D), 0.02),
        "norm2_g": 1.0 + nrm(ks[3], (NL, D), 0.02),
        "normf_g": 1.0 + nrm(ks[4], (D,), 0.02),
        "w_ada": nrm(ks[5], (NL, D, 6 * D), D ** -0.5),
        "b_ada": nrm(ks[6], (NL, 6 * D), 0.01),
        "w_in": nrm(ks[7], (NL, D, IN_WIDTH), D ** -0.5),
        "pe_k": nrm(ks[8], (NL, CMP_BLOCK, HEAD_DIM), 0.02),
        "pe_v": nrm(ks[9], (NL, CMP_BLOCK, HEAD_DIM), 0.02),
        "w_ck1": nrm(ks[10], (NL, CMP_BLOCK * HEAD_DIM, CMP_HIDDEN), (CMP_BLOCK * HEAD_DIM) ** -0.5),
        "w_ck2": nrm(ks[11], (NL, CMP_HIDDEN, HEAD_DIM), CMP_HIDDEN ** -0.5),
        "w_cv1": nrm(ks[12], (NL, CMP_BLOCK * HEAD_DIM, CMP_HIDDEN), (CMP_BLOCK * HEAD_DIM) ** -0.5),
        "w_cv2": nrm(ks[13], (NL, CMP_HIDDEN, HEAD_DIM), CMP_HIDDEN ** -0.5),
        "lam_re": -0.5 + nrm(ks[14], (NL, G, P), 0.01),
        "lam_im": math.pi * jnp.arange(P, dtype=f32) + nrm(ks[15], (NL, G, P), 0.01),
        "log_step": jax.random.uniform(ks[16], (NL, G), f32, math.log(1e-3), math.log(1e-1)),
        "b_re": nrm(ks[17], (NL, G, P, C), (2 * C) ** -0.5),
        "b_im": nrm(ks[18], (NL, G, P, C), (2 * C) ** -0.5),
        "c_re": nrm(ks[19], (NL, G, C, P), P ** -0.5),
        "c_im": nrm(ks[20], (NL, G, C, P), P ** -0.5),
        "d_skip": nrm(ks[21], (NL, G, C), 1.0),
        "w_glu": nrm(ks[22], (NL, SSM_WIDTH, SSM_WIDTH), SSM_WIDTH ** -0.5),
        "b_glu": nrm(ks[23], (NL, SSM_WIDTH), 0.01),
        "attn_norm_g": 1.0 + nrm(ks[24], (NL, ATTN_WIDTH), 0.02),
        "ssm_norm_g": 1.0 + nrm(ks[25], (NL, SSM_WIDTH), 0.02),
        "w_o": nrm(ks[26], (NL, D, D), D ** -0.5),
        "w_up": nrm(ks[27], (NL, D, 2 * F), D ** -0.5),
        "conv_w": nrm(ks[28], (NL, CONV_WIDTH, 2 * F), CONV_WIDTH ** -0.5),
        "conv_b": nrm(ks[29], (NL, 2 * F), 0.01),
        "w_down": nrm(ks[30], (NL, F, D), F ** -0.5),
    }


def reference(x, c, norm1_g, norm2_g, normf_g, w_ada, b_ada, w_in, pe_k, pe_v, w_ck1, w_ck2,
              w_cv1, w_cv2, lam_re, lam_im, log_step, b_re, b_im, c_re, c_im, d_skip, w_glu,
              b_glu, attn_norm_g, ssm_norm_g, w_o, w_up, conv_w, conv_b, w_down):
    b, l, _ = x.shape
    sizes = [ATTN_WIDTH] + [KV_WIDTH] * 6 + [3 * ATTN_HEADS]
    cuts = [int(v) for v in np.cumsum(sizes)]
    for li in range(DEPTH):
        mod = (jax.nn.silu(c) @ w_ada[li] + b_ada[li])[:, None, :]
        sh1, sc1, gt1, sh2, sc2, gt2 = jnp.split(mod, 6, axis=-1)
        h = rmsnorm(x, norm1_g[li]) * (1.0 + sc1) + sh1
        z = h @ w_in[li]
        zq, zkc, zvc, zks, zvs, zkw, zvw, zg, zs = jnp.split(z, cuts, axis=-1)
        gates = jax.nn.sigmoid(zg.reshape(b, l, ATTN_HEADS, 3))
        o_attn = nsa_attention(split_heads(zq, ATTN_HEADS), split_heads(zkc, KV_HEADS),
                               split_heads(zvc, KV_HEADS), split_heads(zks, KV_HEADS),
                               split_heads(zvs, KV_HEADS), split_heads(zkw, KV_HEADS),
                               split_heads(zvw, KV_HEADS), gates, pe_k[li], pe_v[li],
                               w_ck1[li], w_ck2[li], w_cv1[li], w_cv2[li])
        y_ssm = s5_mixer(zs, lam_re[li], lam_im[li], log_step[li], b_re[li], b_im[li],
                         c_re[li], c_im[li], d_skip[li], w_glu[li], b_glu[li])
        mix = jnp.concatenate([rmsnorm(o_attn, attn_norm_g[li]), rmsnorm(y_ssm, ssm_norm_g[li])], axis=-1)
        x = x + gt1 * (mix @ w_o[li])
        h = rmsnorm(x, norm2_g[li]) * (1.0 + sc2) + sh2
        u = causal_dwconv(h @ w_up[li], conv_w[li], conv_b[li])
        a, v = jnp.split(u, 2, axis=-1)
        x = x + gt2 * ((jax.nn.silu(a) * v) @ w_down[li])
    return rmsnorm(x, normf_g)
```

```python
import os
import numpy as np
import ml_dtypes
from contextlib import ExitStack
import concourse.bass as bass
import concourse.mybir as mybir
from concourse.bass_utils import run_bass_kernel_spmd

F32 = mybir.dt.float32
BF16 = mybir.dt.bfloat16
ALU = mybir.AluOpType
AF = mybir.ActivationFunctionType
AX = mybir.AxisListType
ENGS = ["pe", "act", "dve", "pool", "sp"]
NCORE = 8
NSEG = 16
TOK = 2048
D = 1024
EPS = 1e-6
KVW = 12416
O_KS, O_KW, O_KC, O_VC, O_VS, O_VW = 0, 2048, 4096, 6144, 8192, 10304
VE = 66


class Prog:
    def __init__(self, nc, stack, n_dma_sems=80):
        self.nc = nc
        self.cc_pool = [stack.enter_context(nc.semaphore("c%d" % i)) for i in range(4)]
        self.esem = {e: stack.enter_context(nc.semaphore("s_" + e)) for e in ENGS}
        self.ecnt = {e: 0 for e in ENGS}
        self.dma_pool = [stack.enter_context(nc.semaphore("d%d" % i)) for i in range(60)]
        self.sw_pool = [stack.enter_context(nc.semaphore("w%d" % i)) for i in range(24)]
        self.cnts = {}
        self.cc_used = 0
        self.dma_sem = {}
        self.dma_cnt = {}
        self.reset()

    def reset(self):
        self.ops = {e: [] for e in ENGS}
        self.last_w = {}
        self.readers = {}

    def op(self, eng, fn, reads=(), writes=(), dma=None, inc=16):
        deps = set()
        for r in reads:
            w = self.last_w.get(r)
            if w is not None:
                deps.add(w)
            if isinstance(r, tuple) and r[0] == "bank":
                for rd in self.readers.get(r, ()):
                    if rd[0] != eng:
                        deps.add(rd)
        for w_ in writes:
            w = self.last_w.get(w_)
            if w is not None:
                deps.add(w)
            for rd in self.readers.get(w_, ()):
                deps.add(rd)
        me = (eng, len(self.ops[eng]))
        deps.discard(me)
        self.ops[eng].append(dict(fn=fn, deps=deps, dma=dma, sig=dma is not None, val=0, inc=inc, eng=eng))
        for r in reads:
            self.readers.setdefault(r, []).append(me)
        for w_ in writes:
            self.last_w[w_] = me
            self.readers[w_] = []
        return me

    def dma(self, eng, out, in_, reads=(), writes=(), key=None, **kw):
        assert key is not None
        return self.op(eng, lambda e: e.dma_start(out=out, in_=in_, **kw), reads, writes, dma=key)

    def emit(self):
        nc = self.nc
        ops = self.ops
        for e in ENGS:
            for o in ops[e]:
                for (de, di) in o["deps"]:
                    d = ops[de][di]
                    if d["dma"] is None and not (de == "pe" and e == "pe"):
                        d["sig"] = True
        keymap = {}
        nhw = nsw = 0
        for e in ENGS:
            for o in ops[e]:
                if o["dma"] is not None:
                    k = o["dma"]
                    if k not in keymap:
                        if o["inc"] == 1:
                            keymap[k] = self.cc_pool[self.cc_used]
                            self.cc_used += 1
                        elif o["eng"] == "pool":
                            keymap[k] = self.sw_pool[nsw]
                            nsw += 1
                        else:
                            keymap[k] = self.dma_pool[nhw]
                            nhw += 1
                    sem = keymap[k]
                    self.cnts[sem.name] = self.cnts.get(sem.name, 0) + o["inc"]
                    o["val"] = self.cnts[sem.name]
                elif o["sig"]:
                    self.ecnt[e] += 1
                    o["val"] = self.ecnt[e]
        self.dma_sem = keymap
        self.dma_cnt = {k: self.cnts[sem.name] for k, sem in keymap.items()}
        final_dma = dict(self.dma_cnt)
        esem, dma_sem = self.esem, self.dma_sem

        def run(e, engobj, final=False):
            seen = {}
            for o in ops[e]:
                for (de, di) in sorted(o["deps"]):
                    d = ops[de][di]
                    if d["dma"] is not None:
                        sem, val = dma_sem[d["dma"]], d["val"]
                    else:
                        if de == "pe" and e == "pe":
                            continue
                        sem, val = esem[de], d["val"]
                    if seen.get(sem.name, 0) >= val:
                        continue
                    engobj.wait_ge(sem, val)
                    seen[sem.name] = val
                ins = o["fn"](engobj)
                if o["dma"] is not None:
                    if o["inc"] == 1:
                        ins.then_inc(dma_sem[o["dma"]])
                    else:
                        ins.then_inc(dma_sem[o["dma"]], o["inc"])
                elif o["sig"]:
                    ins.then_inc(esem[e], 1)
            if final:
                for k, v in final_dma.items():
                    if seen.get(dma_sem[k].name, 0) < v:
                        engobj.wait_ge(dma_sem[k], v)

        with nc.Block() as block:
            @block.tensor
            def _(t):
                run("pe", t)

            @block.scalar
            def _(s):
                run("act", s)

            @block.vector
            def _(v):
                run("dve", v)

            @block.gpsimd
            def _(g):
                run("pool", g)

            @block.sync
            def _(sp):
                run("sp", sp, final=True)
        self.reset()


def _fm(v, nt):
    return np.ascontiguousarray(np.asarray(v, np.float32).reshape(nt, 128).T)


def _sl(a):
    a = np.asarray(a, np.float32)
    a = a.reshape((16, 2, 64) + a.shape[2:])
    return np.ascontiguousarray(np.moveaxis(a, 0, 2).reshape((128, 16) + a.shape[3:]))


def host_consts(c):
    bf = ml_dtypes.bfloat16
    j = np.arange(NSEG)
    gtok = ((8 * j[:, None] + c) * 128 + np.arange(128)[None, :]).reshape(-1).astype(np.float64)
    inv = np.power(10000.0, -np.arange(32, dtype=np.float32) * 2.0 / 64).astype(np.float32)
    ang = gtok.astype(np.float32)[None, :] * np.concatenate([inv, inv])[:, None]
    cos = np.cos(ang).astype(np.float32)
    sin = np.sin(ang).astype(np.float32)
    sin[:32] *= -1.0
    out = {"cosT": np.concatenate([cos, cos], 0), "sinT": np.concatenate([sin, sin], 0)}
    key = np.arange(128)[:, None, None]
    q = np.arange(128)[None, None, :]
    kt = np.arange(8)[None, :, None]
    out["cmask"] = (((kt - c) * 128 + key - q) <= 0).astype(bf)
    kt = np.arange(12)[None, :, None]
    dl = (kt - 4 - c) * 128 + key - q
    out["wmask"] = ((dl <= 0) & (dl > -512)).astype(bf)
    dd = np.array([0, 1024, 2048])[None, :, None]
    out["cmaskc"] = ((dd + 128 * c + q - 16 * key - 31) >= 0).astype(bf)
    g = 8 * j + c
    cur = 2 * g[None, :, None] + (np.arange(128)[:, None, None] >= 64)
    s = np.arange(256)[None, None, :]
    am = np.zeros((128, NSEG, 256), np.float32)
    am[(s == 0) | (s == cur) | (s == cur - 1)] = 8192.0
    am = np.where(s > cur, -1e30, am)
    out["amask"] = am.astype(bf)
    oh = np.zeros((128, 8), np.float32)
    oh[:, c] = 1.0
    out["onehot"] = oh
    return out


def shared_consts():
    bf = ml_dtypes.bfloat16
    out = {"identF": np.eye(128, dtype=np.float32)}
    sl = np.arange(128)[:, None, None]
    ktl = np.arange(64)[None, :, None]
    key = np.arange(128)[None, None, :]
    out["E_all"] = (sl == 2 * ktl + key // 64).astype(bf)
    n = np.arange(1024)
    s = np.arange(256)
    ov = np.clip(np.minimum(n[:, None] * 16 + 32, s[None, :] * 64 + 64) - np.maximum(n[:, None] * 16, s[None, :] * 64), 0, None) / 32.0
    ove = np.concatenate([ov, np.ones((1024, 2))], 1)
    ove[1023] = 0.0
    out["ov_ext"] = np.ascontiguousarray(ove.reshape(8, 128, 258).transpose(1, 0, 2)).astype(bf)
    b = np.arange(128)
    out["blk16"] = (b[:, None] // 16 == b[None, :] // 16).astype(bf)
    return out


def _kt(a):
    a = np.asarray(a, np.float32)
    nt = a.shape[0] // 128
    return np.ascontiguousarray(a.reshape(nt, 128, a.shape[1]).transpose(1, 0, 2).reshape(128, -1))


def _w1(a):
    a = np.asarray(a, np.float32).reshape(32, 64, 256).transpose(1, 0, 2).reshape(64, -1)
    return np.ascontiguousarray(np.concatenate([a, a], 0))


def host_prep(inp):
    f = lambda a: np.ascontiguousarray(np.asarray(a, np.float32))
    x = f(inp["x"])[0]
    w_in = f(inp["w_in"])[0]
    perm = (np.arange(64) + 32) % 64
    def pc(lo, n):
        idx = np.concatenate([lo + h * 64 + perm for h in range(n)])
        return w_in[:, idx]
    q, kc, vc, ks, vs, kw, vw = (w_in[:, 0:512], w_in[:, 512:640], w_in[:, 640:768], w_in[:, 768:896],
                                 w_in[:, 896:1024], w_in[:, 1024:1152], w_in[:, 1152:1280])
    gt, ssm = w_in[:, 1280:1304], w_in[:, 1304:1816]
    qperm = pc(0, 8)
    hsel = lambda a: np.concatenate([a[:, hh * 64:(hh + 1) * 64] for t in range(4) for hh in (t, 4 + t)], 1)
    wf = np.concatenate([hsel(q), hsel(qperm), kc, pc(512, 2), ks, pc(768, 2), kw, pc(1024, 2), vc, ssm], 1)
    wt = np.concatenate([vs, vw, gt], 1)
    sh = dict(
        cT=_fm(inp["c"][0], 8), b_adaT=_fm(inp["b_ada"][0], 48), g1T=_fm(inp["norm1_g"][0], 8),
        g2T=_fm(inp["norm2_g"][0], 8), gfT=_fm(inp["normf_g"], 8), w_ada=_kt(f(inp["w_ada"])[0]),
        wf=_kt(wf), wt=_kt(wt),
        w_ck1=_w1(inp["w_ck1"][0]), w_cv1=_w1(inp["w_cv1"][0]), w_ck2=_kt(f(inp["w_ck2"])[0]), w_cv2=_kt(f(inp["w_cv2"])[0]),
        pekT=np.ascontiguousarray(np.tile(f(inp["pe_k"])[0].T, (2, 1))), pevT=np.ascontiguousarray(np.tile(f(inp["pe_v"])[0].T, (2, 1))),
        lamre=_sl(inp["lam_re"][0]), lamim=_sl(inp["lam_im"][0]),
        lstep=_sl(np.repeat(np.asarray(inp["log_step"][0])[:, None], 64, 1)),
        bre=_sl(inp["b_re"][0]), bim=_sl(inp["b_im"][0]),
        cre=_sl(np.transpose(np.asarray(inp["c_re"][0]), (0, 2, 1))), cim=_sl(np.transpose(np.asarray(inp["c_im"][0]), (0, 2, 1))),
        dskipT=_fm(np.asarray(inp["d_skip"][0]).reshape(-1), 4), w_glu=_kt(f(inp["w_glu"])[0]), b_gluT=_fm(inp["b_glu"][0], 4),
        agB=np.ascontiguousarray(np.tile(f(inp["attn_norm_g"])[0][None, :], (128, 1))), sgT=_fm(inp["ssm_norm_g"][0], 4),
        w_o=_kt(f(inp["w_o"])[0]), w_up=_kt(f(inp["w_up"])[0]),
        conv_wT=np.ascontiguousarray(f(inp["conv_w"])[0].reshape(3, 44, 128).transpose(2, 0, 1)),
        conv_bT=_fm(inp["conv_b"][0], 44), w_down=_kt(f(inp["w_down"])[0]),
    )
    sh.update(shared_consts())
    maps = []
    xs = x.reshape(NSEG, NCORE, 128, D)
    for c in range(NCORE):
        m = dict(sh)
        m["x_c"] = np.ascontiguousarray(xs[:, c].transpose(1, 0, 2).reshape(128, NSEG * D))
        m.update(host_consts(c))
        maps.append({"inp": pack_inputs(m)})
    return maps


IN_COLS = [
    ("x_c", 16384), ("cT", 8), ("b_adaT", 48), ("g1T", 8), ("g2T", 8), ("gfT", 8), ("w_ada", 49152), ("wf", 19456), ("wt", 2240),
    ("w_ck1", 8192), ("w_cv1", 8192), ("w_ck2", 128), ("w_cv2", 128), ("pekT", 32), ("pevT", 32),
    ("lamre", 16), ("lamim", 16), ("lstep", 16), ("bre", 256), ("bim", 256), ("cre", 256), ("cim", 256),
    ("dskipT", 4), ("w_glu", 2048), ("b_gluT", 4), ("agB", 512), ("sgT", 4), ("w_o", 8192), ("w_up", 45056),
    ("conv_wT", 132), ("conv_bT", 44), ("w_down", 22528), ("identF", 128), ("E_all", 8192), ("ov_ext", 2064),
    ("cosT", 2048), ("sinT", 2048), ("cmask", 1024), ("wmask", 1536), ("cmaskc", 384), ("amask", 4096), ("onehot", 8),
]
IN_TOT = sum(c for _, c in IN_COLS)


def pack_inputs(m):
    parts = []
    for n, c in IN_COLS:
        a = np.asarray(m[n]).astype(np.float32).reshape(128, -1)
        assert a.shape[1] == c, (n, a.shape, c)
        parts.append(a)
    return np.ascontiguousarray(np.concatenate(parts, 1))


class K:
    pass


def build(stop_after=99, dbg_cols=0):
    nc = bass.Bass("TRN2", target_bir_lowering=False)
    k = K()
    k.nc = nc
    k.kvin = nc.dram_tensor("kvin", [128, KVW], BF16).ap()
    k.kvall = nc.dram_tensor("kvall", [NCORE * 128, KVW], BF16).ap()
    inp = nc.dram_tensor("inp", [128, IN_TOT], F32, kind="ExternalInput").ap()
    k.I = {}
    c0 = 0
    for n, c in IN_COLS:
        k.I[n] = inp[:, c0:c0 + c]
        c0 += c
    k.out = nc.dram_tensor("out", [TOK, D], F32, kind="ExternalOutput").ap()
    k.dbg = nc.dram_tensor("dbg", [128, dbg_cols], F32, kind="ExternalOutput").ap() if dbg_cols else None
    k.stop_after = stop_after
    k.lin = nc.dram_tensor("lin", [128, 512], F32).ap()
    k.lall = nc.dram_tensor("lall", [NCORE * 128, 512], F32).ap()
    with ExitStack() as st:
        k.P = Prog(nc, st)
        k.sb = lambda name, shape, dt, s=st: s.enter_context(nc.sbuf_tensor("sb_" + name, shape, dt))
        k.modT = k.sb("modT", [128, 48], F32)
        k.g1s = k.sb("g1s", [128, 8], F32)
        k.g2s = k.sb("g2s", [128, 8], F32)
        k.identF = k.sb("identF", [128, 128], F32)
        k.identB = k.sb("identB", [128, 128], BF16)
        k.onesB = k.sb("onesB", [128, 128], BF16)
        k.psum = [st.enter_context(nc.psum_tensor("ps%d" % i, [128, 512], F32)) for i in range(8)]
        k.x1d = nc.dram_tensor("x1d", [128, 8, TOK], F32).ap()
        k.hin = nc.dram_tensor("hin", [128, 256], BF16).ap()
        k.hall = nc.dram_tensor("hall", [NCORE * 128, 256], BF16).ap()
        k.qz = k.sb("qz", [128, 8, TOK], BF16)
        with ExitStack() as st1:
            sb1 = lambda name, shape, dt: st1.enter_context(nc.sbuf_tensor("sb_" + name, shape, dt))
            k.mixT = sb1("mixT", [128, 8, TOK], BF16)
            with ExitStack() as st2:
                sb2 = lambda name, shape, dt: st2.enter_context(nc.sbuf_tensor("sb_" + name, shape, dt))
                k.qT = k.qz[:, 0:4, :]
                k.zsT = k.qz[:, 4:8, :]
                k.gates = sb2("gates", [128, NSEG, 24], F32)
                k.kcmpT = sb2("kcmpT", [128, 1024], BF16)
                k.vcmp = sb2("vcmp", [128, 8, 2, VE], BF16)
                phase0(k)
                if stop_after >= 1:
                    phase1(k)
                if stop_after >= 2:
                    phase2(k)
                if stop_after >= 3 and not os.environ.get("SKIP_ATT"):
                    phase3(k)
                if stop_after >= 4:
                    phase_ssm(k)
            if stop_after >= 5:
                phase4(k, st1)
    return nc


def ld(k, dst, src, tok, key, eng="sp"):
    k.P.dma(eng, dst, src, writes=[tok], key=key)


def phase0(k):
    nc, P, I = k.nc, k.P, k.I
    with ExitStack() as st:
        sb = lambda name, shape, dt: st.enter_context(nc.sbuf_tensor("sb_" + name, shape, dt))
        wa = sb("wa", [128, 8, 6144], BF16)
        cT = sb("cT", [128, 8], F32)
        scb = sb("scb", [128, 8, 2], BF16)
        bT = sb("bT", [128, 48], F32)
        g1T = sb("g1T", [128, 8], F32)
        g2T = sb("g2T", [128, 8], F32)
        if os.environ.get("AG_FIRST"):
            P.op("pool", lambda e: e.collective_compute("AllGather", ALU.bypass, replica_groups=[list(range(NCORE))],
                                                        ins=[k.kvin.opt()], outs=[k.kvall.opt()]),
                 writes=["kvall"], dma="cc0", inc=1)
        ld(k, cT[:], I["cT"], "cT", "l0")
        ld(k, bT[:], I["b_adaT"], "bT", "l1")
        ld(k, g1T[:], I["g1T"], "g1T", "l2")
        ld(k, g2T[:], I["g2T"], "g2T", "l3")
        ld(k, k.identF[:], I["identF"], "identF", "l4")
        for kt in range(0 if not os.environ.get("P0_SKIP_DMA") else 8, 8):
            P.dma("pool", wa[:, kt, :], I["w_ada"][:, kt * 6144:(kt + 1) * 6144], writes=[("wa", kt)], key=("wa", kt),
                  max_dma_last_dim=8192)
        P.op("dve", lambda e: e.memset(k.onesB[:], 1.0), writes=["onesB"])
        P.op("dve", lambda e: e.tensor_copy(out=k.identB[:], in_=k.identF[:]), reads=["identF"], writes=["identB"])
        for dup in range(2):
            P.op("act", lambda e, dup=dup: e.activation(out=scb[:, :, dup], in_=cT[:], func=AF.Silu), reads=["cT"], writes=["scb"])
        pm = k.psum[0]
        for m in list(range(0 if not os.environ.get("P0_SKIP_MM") else 48, 48)) * int(os.environ.get("P0_REP", "1")):
            for kt in range(8):
                P.op("pe", lambda e, m=m, kt=kt: e.matmul(pm[:, 2 * m:2 * m + 2], lhsT=wa[:, kt, m * 128:(m + 1) * 128],
                                                          rhs=scb[:, kt, :], start=(kt == 0), stop=(kt == 7)),
                     reads=["scb", ("wa", kt)], writes=[("bank", 0)])
        P.op("dve", lambda e: e.tensor_tensor(out=k.modT[:], in0=pm[:, 0:96:2], in1=bT[:], op=ALU.add),
             reads=[("bank", 0), "bT"], writes=["modT"])
        P.op("dve", lambda e: e.scalar_tensor_tensor(out=k.g1s[:], in0=k.modT[:, 8:16], scalar=1.0, in1=g1T[:],
                                                     op0=ALU.add, op1=ALU.mult), reads=["modT", "g1T"], writes=["g1s"])
        P.op("dve", lambda e: e.scalar_tensor_tensor(out=k.g2s[:], in0=k.modT[:, 32:40], scalar=1.0, in1=g2T[:],
                                                     op0=ALU.add, op1=ALU.mult), reads=["modT", "g2T"], writes=["g2s"])
        P.emit()


def rmsnorm_T(k, st_tok, src, dst, gain, shift, sq, rstd, tmp, bank, nfeat=1024.0):
    P = k.P
    nsq = sq.shape[1]
    for ft in range(8):
        P.op("act", lambda e, ft=ft: e.activation(out=sq[:, ft % nsq, :], in_=src[ft], func=AF.Square),
             reads=[st_tok + ("src", ft)], writes=[("sq", ft % nsq)])
        P.op("pe", lambda e, ft=ft: e.matmul(bank[:], lhsT=k.onesB[:], rhs=sq[:, ft % nsq, :], start=(ft == 0), stop=(ft == 7)),
             reads=[("sq", ft % nsq), "onesB"], writes=[("bank", 7)])
    P.op("act", lambda e: e.activation(out=rstd[:], in_=bank[:], func=AF.Sqrt, scale=1.0 / nfeat, bias=EPS),
         reads=[("bank", 7)], writes=["rstd"])
    P.op("dve", lambda e: e.reciprocal(out=rstd[:], in_=rstd[:]), reads=["rstd"], writes=["rstd"])
    for ft in range(8):
        P.op("dve", lambda e, ft=ft: e.tensor_tensor(out=tmp[:, ft % tmp.shape[1], :], in0=src[ft], in1=rstd[:], op=ALU.mult),
             reads=[st_tok + ("src", ft), "rstd"], writes=[("ntmp", ft % tmp.shape[1])])
        if shift is not None:
            P.op("act", lambda e, ft=ft: e.activation(out=dst[ft], in_=tmp[:, ft % tmp.shape[1], :], func=AF.Identity,
                                                      scale=gain[:, ft:ft + 1], bias=shift[:, ft:ft + 1]),
                 reads=[("ntmp", ft % tmp.shape[1]), "modT", "g1s", "g2s", "gfT"], writes=[st_tok + ("dst", ft)])
        else:
            P.op("act", lambda e, ft=ft: e.activation(out=dst[ft], in_=tmp[:, ft % tmp.shape[1], :], func=AF.Copy,
                                                      scale=gain[:, ft:ft + 1]),
                 reads=[("ntmp", ft % tmp.shape[1]), "gfT"], writes=[st_tok + ("dst", ft)])


def phase1(k):
    nc, P, I = k.nc, k.P, k.I
    with ExitStack() as st:
        sb = lambda name, shape, dt: st.enter_context(nc.sbuf_tensor("sb_" + name, shape, dt))
        wf = sb("wf", [128, 8, 2432], BF16)
        wt = sb("wt", [128, 8, 280], BF16)
        cosT = sb("cosT", [128, TOK], F32)
        sinT = sb("sinT", [128, TOK], F32)
        xtok = sb("xtok", [128, 4, D], F32)
        xT = sb("xT", [128, 8, 512], F32)
        hT = sb("hT", [128, 8, 512], BF16)
        sq = sb("sq", [128, 8, 512], BF16)
        rstd = sb("rstd", [128, 512], F32)
        ntmp = sb("ntmp", [128, 2, 512], F32)
        t1 = sb("t1", [128, 2, 512], F32)
        t2 = sb("t2", [128, 2, 512], F32)
        kst = sb("kst", [128, 4, 512], BF16)
        vst = sb("vst", [128, 2, 4, 2 * VE], BF16)
        for kt in range(8):
            P.dma("pool", wf[:, kt, :], I["wf"][:, kt * 2432:(kt + 1) * 2432], writes=[("wf", kt)], key=("wf", kt),
                  max_dma_last_dim=8192)
            P.dma("pool", wt[:, kt, :], I["wt"][:, kt * 280:(kt + 1) * 280], writes=[("wt", kt)], key=("wt", kt))
        ld(k, cosT[:], I["cosT"], "cosT", "l0")
        ld(k, sinT[:], I["sinT"], "sinT", "l1")
        P.op("pool", lambda e: e.memset(vst[:], 1.0), writes=[("vst", 0), ("vst", 1)])
        ps = k.psum
        bi = [0]

        def bank():
            b = bi[0] % int(os.environ.get("NBANK", "6"))
            bi[0] += 1
            return ps[b], ("bank", b)

        def do_tg(tg):
            ts = slice(tg * 512, (tg + 1) * 512)
            P.dma("sp", xtok[:], I["x_c"][:, tg * 4096:(tg + 1) * 4096].rearrange("p (s d) -> p s d", s=4),
                  writes=["xtok"], key="xtok")
            for ft in range(8):
                b, bt = bank()
                for s in range(4):
                    P.op("pe", lambda e, b=b, s=s, ft=ft: e.transpose(out=b[:, s * 128:(s + 1) * 128],
                                                                      in_=xtok[:, s, ft * 128:(ft + 1) * 128], identity=k.identF[:]),
                         reads=["xtok", "identF"], writes=[bt])
                P.op("act", lambda e, b=b, ft=ft: e.copy(out=xT[:, ft, :], in_=b[:]), reads=[bt], writes=[("x", "src", ft)])
            CUT = float(os.environ.get("P1_CUT", "9"))
            if CUT < 1:
                return
            rmsnorm_T(k, ("x",), [xT[:, ft, :] for ft in range(8)], [hT[:, ft, :] for ft in range(8)],
                      k.g1s, k.modT[:, 0:8], sq, rstd, ntmp, ps[7])
            if CUT < 2:
                return
            hreads = [("x", "dst", ft) for ft in range(8)]

            def proj(ct, b, bt):
                for kt in range(8):
                    P.op("pe", lambda e, kt=kt: e.matmul(b[:], lhsT=wf[:, kt, ct * 128:(ct + 1) * 128], rhs=hT[:, kt, :],
                                                         start=(kt == 0), stop=(kt == 7)),
                         reads=[("x", "dst", kt), ("wf", kt)], writes=[bt])

            def rope_pair(ct, ctp, dst, dtok, slot):
                b1, bt1 = bank()
                proj(ct, b1, bt1)
                b2, bt2 = bank()
                proj(ctp, b2, bt2)
                P.op("dve", lambda e: e.tensor_tensor(out=t1[:, slot, :], in0=b1[:], in1=cosT[:, ts], op=ALU.mult),
                     reads=[bt1, "cosT"], writes=[("t1", slot)])
                P.op("dve", lambda e: e.tensor_tensor(out=t2[:, slot, :], in0=b2[:], in1=sinT[:, ts], op=ALU.mult),
                     reads=[bt2, "sinT"], writes=[("t2", slot)])
                P.op("pool", lambda e: e.tensor_tensor(out=dst, in0=t1[:, slot, :], in1=t2[:, slot, :], op=ALU.add),
                     reads=[("t1", slot), ("t2", slot)], writes=[dtok])

            for qi in range(4 if not os.environ.get("NOQ") else 0):
                rope_pair(qi, 4 + qi, k.qT[:, qi, ts], ("qT", qi, tg), qi % 2)
            if CUT < 3:
                return
            for i, (ct, off) in enumerate([(8, O_KC), (10, O_KS), (12, O_KW)] if not os.environ.get("NOK") else []):
                rope_pair(ct, ct + 1, kst[:, i, :], ("kst", i), i % 2)
                if not os.environ.get("NOKV"):
                    P.dma("sp", k.kvin[:, off + tg * 512: off + (tg + 1) * 512], kst[:, i, :], reads=[("kst", i)],
                          writes=[("kvin", off, tg)], key=("kst", i))
            if CUT < 3.3:
                return
            b, bt = bank()
            proj(14, b, bt)
            P.op("act", lambda e, b=b: e.copy(out=kst[:, 3, :], in_=b[:]), reads=[bt], writes=[("kst", 3)])
            if not os.environ.get("NOKV"):
                P.dma("sp", k.kvin[:, O_VC + tg * 512: O_VC + (tg + 1) * 512], kst[:, 3, :], reads=[("kst", 3)],
                      writes=[("kvin", O_VC, tg)], key=("kst", 3))
            if CUT < 3.6:
                return
            for si in range(4):
                b, bt = bank()
                proj((15 + si) if not os.environ.get("ZSALT") else 14, b, bt)
                if os.environ.get("ZSDVE"):
                    P.op("dve", lambda e, b=b, si=si: e.tensor_copy(out=k.zsT[:, si, ts], in_=b[:]), reads=[bt], writes=[("zsT", si, tg)])
                else:
                    P.op("act", lambda e, b=b, si=si: e.copy(out=k.zsT[:, si, ts], in_=b[:]), reads=[bt], writes=[("zsT", si, tg)])
            if CUT < 4:
                return
            for s in range(4):
                b, bt = bank()
                for kt in range(8):
                    P.op("pe", lambda e, kt=kt, s=s, b=b: e.matmul(b[:, 0:280], lhsT=hT[:, kt, s * 128:(s + 1) * 128], rhs=wt[:, kt, :],
                                                                   start=(kt == 0), stop=(kt == 7)),
                         reads=[("x", "dst", kt), ("wt", kt)], writes=[bt])
                for vi in range(2):
                    P.op("dve", lambda e, b=b, s=s, vi=vi: e.tensor_copy(
                        out=vst[:, vi, s, :].rearrange("p (h e) -> p h e", h=2)[:, :, 0:64],
                        in_=b[:, vi * 128:(vi + 1) * 128].rearrange("p (h d) -> p h d", h=2)),
                         reads=[bt], writes=[("vst", vi)])
                P.op("act", lambda e, b=b, s=s: e.activation(out=k.gates[:, tg * 4 + s, :], in_=b[:, 256:280], func=AF.Sigmoid),
                     reads=[bt], writes=[("gates", tg * 4 + s)])
            for vi, off in enumerate([O_VS, O_VW]):
                P.dma("sp", k.kvin[:, off + tg * 8 * VE: off + (tg + 1) * 8 * VE], vst[:, vi, :, :].rearrange("p s e -> p (s e)"),
                      reads=[("vst", vi)], writes=[("kvin", off, tg)], key=("vst", vi))
        for tg in range(4):
            do_tg(tg)
        if k.dbg is not None and k.stop_after == 1:
            P.dma("pool", k.dbg[:, 0:2048], k.qT[:, 1, :], reads=[("qT", 1, t) for t in range(4)], key="dbg")
            P.dma("pool", k.dbg[:, 2048:4096], k.zsT[:, 2, :], reads=[("zsT", 2, t) for t in range(4)], key="dbg1")
            P.dma("sp", k.dbg[:, 4096:4096 + 48], k.modT[:], reads=["modT"], key="dbg2")
            P.dma("sp", k.dbg[:, 4200:4200 + 384], k.gates[:].rearrange("p s g -> p (s g)"), reads=[("gates", i) for i in range(16)], key="dbg3")
        P.emit()


def gload(k, dst, off, width, tokbase, keybase, eng="sp"):
    for c in range(NCORE):
        k.P.dma(eng, dst[:, 0:16384].rearrange("p (j c r) -> p j c r", j=NSEG, c=NCORE)[:, :, c, :],
                k.kvall[c * 128:(c + 1) * 128, off:off + width].rearrange("p (j r) -> p j r", j=NSEG),
                reads=["kvall"], writes=[(tokbase, c)], key=(keybase, c))


def phase2(k):
    nc, P, I = k.nc, k.P, k.I
    with ExitStack() as st:
        sb = lambda name, shape, dt: st.enter_context(nc.sbuf_tensor("sb_" + name, shape, dt))
        xall = [sb("kcall", [128, 16416], BF16), sb("vcall", [128, 16416], BF16)]
        w1 = [sb("w1k", [128, 32, 256], BF16), sb("w1v", [128, 32, 256], BF16)]
        w2 = [sb("w2k", [128, 2, 64], BF16), sb("w2v", [128, 2, 64], BF16)]
        pe2 = [sb("pek2", [128, 32, 2], BF16), sb("pev2", [128, 32, 2], BF16)]
        pef = sb("pef", [128, 2, 32], F32)
        hid = [sb("hid0", [128, 2, 1024], BF16), sb("hid1", [128, 2, 1024], BF16)]
        bias = sb("cbias", [128, 4], F32)
        ps = k.psum
        for kv, (off, wn1, wn2, pn) in enumerate([(O_KC, "w_ck1", "w_ck2", "pekT"), (O_VC, "w_cv1", "w_cv2", "pevT")]):
            P.dma("pool", w1[kv][:], I[wn1].rearrange("p (j h) -> p j h", j=32), writes=[("w1", kv)], key=("w1", kv))
            P.dma("pool", w2[kv][:], I[wn2].rearrange("p (t d) -> p t d", t=2), writes=[("w2", kv)], key=("w2", kv))
            ld(k, pef[:, kv, :], I[pn], ("pef", kv), ("pef", kv))
            for dup in range(2):
                P.op("dve", lambda e, kv=kv, dup=dup: e.tensor_copy(out=pe2[kv][:, :, dup], in_=pef[:, kv, :]),
                     reads=[("pef", kv)], writes=[("pe2", kv)])
        P.op("pool", lambda e: e.memset(k.vcmp[:], 1.0), writes=["vcmp"])
        for h in range(2):
            P.op("pool", lambda e, h=h: e.memset(hid[h][:], 0.0), writes=[("hid", h)])
        if not os.environ.get("SKIP_AG"):
            P.op("pool", lambda e: e.collective_compute("AllGather", ALU.bypass, replica_groups=[list(range(NCORE))],
                                                        ins=[k.kvin.opt()], outs=[k.kvall.opt()]),
                 writes=["kvall"], dma="cc", inc=1)
        for kv, off in enumerate([O_KC, O_VC]):
            gload(k, xall[kv], off, 2048, ("xall", kv), ("xall", kv))
        for kv in range(2):
            for ht in range(2):
                for j in range(32):
                    P.op("pe", lambda e, kv=kv, ht=ht, j=j: e.matmul(ps[6][:, 0:2], lhsT=w1[kv][0:64, j, ht * 128:(ht + 1) * 128],
                                                                      rhs=pe2[kv][0:64, j, :], start=(j == 0), stop=(j == 31)),
                         reads=[("w1", kv), ("pe2", kv)], writes=[("bank", 6)])
                P.op("dve", lambda e, kv=kv, ht=ht: e.tensor_copy(out=bias[:, kv * 2 + ht:kv * 2 + ht + 1], in_=ps[6][:, 0:1]),
                     reads=[("bank", 6)], writes=["cbias"])
        bsel = [0]
        for kv in range(2):
            for ht in range(2):
                for ch in range(2):
                    n0 = ch * 512
                    cnt = 512 if ch == 0 else 511
                    bb = [(ps[bsel[0] % 2 * 2 + h], ("bank", bsel[0] % 2 * 2 + h)) for h in range(2)]
                    bsel[0] += 1
                    for j in range(32):
                        for h in range(2):
                            st_ = j + 16 * n0
                            P.op("pe", lambda e, kv=kv, ht=ht, j=j, h=h, st_=st_, cnt=cnt, bb=bb: e.matmul(
                                bb[h][0][:, 0:cnt], lhsT=w1[kv][h * 64:(h + 1) * 64, j, ht * 128:(ht + 1) * 128],
                                rhs=xall[kv][h * 64:(h + 1) * 64, st_:st_ + 16 * (cnt - 1) + 1:16], start=(j == 0), stop=(j == 31)),
                                 reads=[("w1", kv)] + [(("xall", kv), c) for c in range(NCORE)], writes=[bb[h][1]])
                    for h in range(2):
                        P.op("act", lambda e, kv=kv, ht=ht, h=h, n0=n0, cnt=cnt, bb=bb: e.activation(
                            out=hid[h][:, ht, n0:n0 + cnt], in_=bb[h][0][:, 0:cnt], func=AF.Gelu_apprx_tanh,
                            bias=bias[:, kv * 2 + ht:kv * 2 + ht + 1]),
                             reads=[bb[h][1], "cbias"], writes=[("hid", h)])
            for h in range(2):
                if kv == 0:
                    for ch in range(2):
                        for ht in range(2):
                            P.op("pe", lambda e, h=h, ch=ch, ht=ht: e.matmul(ps[4][h * 64:(h + 1) * 64, :], lhsT=w2[0][:, ht, :],
                                                                             rhs=hid[h][:, ht, ch * 512:(ch + 1) * 512], start=(ht == 0), stop=(ht == 1)),
                                 reads=[("w2", 0), ("hid", h)], writes=[("bank", 4)])
                        P.op("dve", lambda e, h=h, ch=ch: e.tensor_copy(out=k.kcmpT[h * 64:(h + 1) * 64, ch * 512:(ch + 1) * 512],
                                                                        in_=ps[4][h * 64:(h + 1) * 64, :]),
                             reads=[("bank", 4)], writes=["kcmpT"])
                else:
                    for nt in range(8):
                        for ht in range(2):
                            P.op("pe", lambda e, h=h, nt=nt, ht=ht: e.matmul(ps[5][:, 0:64], lhsT=hid[h][:, ht, nt * 128:(nt + 1) * 128],
                                                                             rhs=w2[1][:, ht, :], start=(ht == 0), stop=(ht == 1)),
                                 reads=[("w2", 1), ("hid", h)], writes=[("bank", 5)])
                        P.op("dve", lambda e, h=h, nt=nt: e.tensor_copy(out=k.vcmp[:, nt, h, 0:64], in_=ps[5][:, 0:64]),
                             reads=[("bank", 5)], writes=["vcmp"])
        P.emit()


def phase3(k):
    nc, P, I = k.nc, k.P, k.I
    NJ = int(os.environ.get("ATT_J", NSEG))
    with ExitStack() as st:
        sb = lambda name, shape, dt: st.enter_context(nc.sbuf_tensor("sb_" + name, shape, dt))
        ksT = sb("ksTall", [128, 16384], BF16)
        vsA = sb("vsall", [128, 128, 2 * VE], BF16)
        E_all = sb("E_all", [128, 64, 128], BF16)
        ov = sb("ov", [128, 8, 258], BF16)
        cmask = sb("cmask", [128, 8, 128], BF16)
        wmask = sb("wmask", [128, 12, 128], BF16)
        cmaskc = sb("cmaskc", [128, 3, 128], BF16)
        amask = sb("amask", [128, NSEG, 256], BF16)
        agB = sb("agB", [128, 512], F32)
        kwT = [sb("kwT%d" % i, [128, 12, 128], BF16) for i in range(2)]
        vwB = [sb("vwB%d" % i, [128, 12, 2 * VE], BF16) for i in range(2)]
        pT = [sb("pT%d" % i, [128, 512], BF16) for i in range(2)]
        pTm = [sb("pTm%d" % i, [128, 512], BF16) for i in range(2)]
        msb = [sb("msb%d" % i, [128, 128], BF16) for i in range(2)]
        obr = [sb("obr%d" % i, [128, 512], F32) for i in range(3)]
        impS = sb("impS", [128, 256], F32)
        sc = sb("sc", [128, 256], F32)
        sc2 = sb("sc2", [128, 256], F32)
        m8 = sb("m8", [128, 16], F32)
        selF = sb("selF", [128, 256], F32)
        selT = sb("selT", [128, 2, 128], BF16)
        small = sb("small", [128, 32], F32)
        oacc = sb("oacc", [128, 8, 64], F32)
        sqt = sb("sqt", [128, 8, 64], F32)
        mixtok = sb("mixtok", [128, 512], F32)
        ps = k.psum
        for br_ in range(3):
            P.op("pool", lambda e, br_=br_: e.memset(obr[br_][:], 0.0), writes=[("obr", br_)])
        gload(k, ksT, O_KS, 2048, "ksT", "ksT")
        for c in range(NCORE):
            P.dma("sp", vsA[:].rearrange("p (j c) e -> p j c e", c=NCORE)[:, :, c, :],
                  k.kvall[c * 128:(c + 1) * 128, O_VS:O_VS + NSEG * 2 * VE].rearrange("p (j e) -> p j e", j=NSEG),
                  reads=["kvall"], writes=[("vsA", c)], key=("vsA", c))
        P.dma("pool", E_all[:], I["E_all"].rearrange("p (a b) -> p a b", a=64), writes=["E_all"], key="c0")
        P.dma("pool", ov[:], I["ov_ext"].rearrange("p (a b) -> p a b", a=8), writes=["ov"], key="c1")
        P.dma("pool", cmask[:], I["cmask"].rearrange("p (a b) -> p a b", a=8), writes=["cmask"], key="c2")
        P.dma("pool", wmask[:], I["wmask"].rearrange("p (a b) -> p a b", a=12), writes=["wmask"], key="c3")
        P.dma("pool", cmaskc[:], I["cmaskc"].rearrange("p (a b) -> p a b", a=3), writes=["cmaskc"], key="c4")
        P.dma("pool", amask[:], I["amask"].rearrange("p (a b) -> p a b", a=NSEG), writes=["amask"], key="c5")
        ld(k, agB[:], I["agB"], "agB", "c6")
        ks_reads = [("ksT", c) for c in range(NCORE)]
        vs_reads = [("vsA", c) for c in range(NCORE)]
        cnt = [0]

        def unit(j, h, lhsT_k, kreads, lhsT_v, vreads, mask_ap, mreads, first, last, imp_nt=None):
            u = cnt[0]
            cnt[0] += 1
            sb_, sbt = ps[u % 2], ("bank", u % 2)
            P.op("pe", lambda e: e.matmul(sb_[:], lhsT=lhsT_k, rhs=k.qT[64 * h:64 * h + 64, :, j * 128:(j + 1) * 128],
                                          start=True, stop=True), reads=kreads + [("qT", i, j // 4) for i in range(4)], writes=[sbt])
            p_ = pT[u % 2]
            P.op("act", lambda e: e.activation(out=p_[:], in_=sb_[:], func=AF.Exp, scale=0.125, bias=-8.0),
                 reads=[sbt], writes=[("pT", u % 2)])
            if mask_ap is not None:
                pm_ = pTm[u % 2]
                P.op("dve", lambda e: e.tensor_tensor(out=pm_[:].rearrange("p (a q) -> p a q", a=4),
                                                      in0=p_[:].rearrange("p (a q) -> p a q", a=4),
                                                      in1=mask_ap.unsqueeze(1).to_broadcast([128, 4, 128]), op=ALU.mult),
                     reads=[("pT", u % 2)] + mreads, writes=[("pTm", u % 2)])
                pfin, ptok = pm_, ("pTm", u % 2)
            else:
                pfin, ptok = p_, ("pT", u % 2)
            def back():
                P.op("pe", lambda e: e.matmul(ps[4][0:65, :], lhsT=lhsT_v, rhs=pfin[:], start=first, stop=last),
                     reads=vreads + [ptok], writes=[("bank", 4)])
                if imp_nt is not None:
                    for i in range(4):
                        P.op("pe", lambda e, i=i: e.matmul(ps[5 + i // 2][:, (i % 2) * 256:(i % 2) * 256 + 256], lhsT=pfin[:, i * 128:(i + 1) * 128],
                                                           rhs=ov[:, imp_nt, 0:256], start=(first and i % 2 == 0), stop=last,
                                                           skip_group_check=True),
                             reads=[ptok, "ov"], writes=[("bank", 5 + i // 2)])
            prev = pend[0]
            pend[0] = back
            if prev is not None:
                prev()

        pend = [None]

        def flush():
            if pend[0] is not None:
                pend[0]()
                pend[0] = None

        def evac(br):
            flush()
            P.op("act", lambda e: e.copy(out=obr[br][0:65, :], in_=ps[4][0:65, :]), reads=[("bank", 4)], writes=[("obr", br)])

        def do_seg(j):
            wb = j % 2
            lo = 4 if j == 0 else 0
            kva = k.kvall.rearrange("(c p) w -> p c w", p=128)
            if j > 0:
                P.dma("sp", kwT[wb][:, 0:4, :], kva[:, 4:8, O_KW + (j - 1) * 128:O_KW + j * 128], reads=["kvall"],
                      writes=[("kwT", wb)], key=("kw", wb, 0))
                P.dma("sp", vwB[wb][:, 0:4, :], kva[:, 4:8, O_VW + (j - 1) * 2 * VE:O_VW + j * 2 * VE], reads=["kvall"],
                      writes=[("vwB", wb)], key=("vw", wb, 0))
            P.dma("sp", kwT[wb][:, 4:12, :], kva[:, 0:8, O_KW + j * 128:O_KW + (j + 1) * 128], reads=["kvall"],
                  writes=[("kwT", wb)], key=("kw", wb, 1))
            P.dma("sp", vwB[wb][:, 4:12, :], kva[:, 0:8, O_VW + j * 2 * VE:O_VW + (j + 1) * 2 * VE], reads=["kvall"],
                  writes=[("vwB", wb)], key=("vw", wb, 1))
            for h in range(2):
                ntm = j // 2
                for nt in range(ntm + 1):
                    dlt = 1024 * j - 2048 * nt
                    mk = cmaskc[:, dlt // 1024, :] if dlt <= 2048 else None
                    unit(j, h, k.kcmpT[64 * h:64 * h + 64, nt * 128:(nt + 1) * 128], ["kcmpT"], k.vcmp[:, nt, h, 0:65], ["vcmp"],
                         mk, ["cmaskc"], nt == 0, nt == ntm, imp_nt=nt)
                evac(0)
                for i in range(4):
                    P.op("dve", lambda e, i=i: e.reduce_sum(out=small[:, i:i + 1], in_=ps[5 + i // 2][:, (i % 2) * 256:(i % 2) * 256 + 256], axis=AX.X),
                         reads=[("bank", 5 + i // 2)], writes=["small"])
                P.op("dve", lambda e: e.tensor_scalar(out=small[:, 0:4], in0=small[:, 0:4], scalar1=1e-30, scalar2=None, op0=ALU.max),
                     reads=["small"], writes=["small"])
                P.op("dve", lambda e: e.reciprocal(out=small[:, 4:8], in_=small[:, 0:4]), reads=["small"], writes=["small"])
                P.op("dve", lambda e: e.tensor_scalar(out=impS[:], in0=ps[5][:, 0:256], scalar1=small[:, 4:5], scalar2=None, op0=ALU.mult),
                     reads=[("bank", 5), "small"], writes=["impS"])
                for i in range(1, 4):
                    P.op("dve", lambda e, i=i: e.scalar_tensor_tensor(out=impS[:], in0=ps[5 + i // 2][:, (i % 2) * 256:(i % 2) * 256 + 256],
                                                                      scalar=small[:, 4 + i:5 + i], in1=impS[:], op0=ALU.mult, op1=ALU.add),
                         reads=[("bank", 5 + i // 2), "small", "impS"], writes=["impS"])
                P.op("dve", lambda e: e.tensor_tensor(out=sc[:], in0=impS[:], in1=amask[:, j, :], op=ALU.add),
                     reads=["impS", "amask"], writes=["sc"])
                P.op("dve", lambda e: e.max(out=m8[:, 0:8], in_=sc[:]), reads=["sc"], writes=["m8"])
                P.op("dve", lambda e: e.match_replace(out=sc2[:], in_to_replace=m8[:, 0:8], in_values=sc[:], imm_value=-1e30),
                     reads=["sc", "m8"], writes=["sc2"])
                P.op("dve", lambda e: e.max(out=m8[:, 8:16], in_=sc2[:]), reads=["sc2"], writes=["m8"])
                P.op("dve", lambda e: e.tensor_scalar(out=selF[:], in0=sc[:], scalar1=m8[:, 15:16], scalar2=None, op0=ALU.is_ge),
                     reads=["sc", "m8"], writes=["selF"])
                P.op("dve", lambda e: e.scalar_tensor_tensor(out=selF[:], in0=sc[:], scalar=-1e29, in1=selF[:], op0=ALU.is_gt, op1=ALU.mult),
                     reads=["sc", "selF"], writes=["selF"])
                for t in range(2):
                    P.op("pe", lambda e, t=t: e.transpose(out=ps[7][:, t * 128:(t + 1) * 128], in_=selF[:, t * 128:(t + 1) * 128], identity=k.identF[:]),
                         reads=["selF", "identF"], writes=[("bank", 7)])
                P.op("act", lambda e: e.copy(out=selT[:].rearrange("p t q -> p (t q)"), in_=ps[7][:, 0:256]), reads=[("bank", 7)], writes=["selT"])
                nkt = 8 * j + 8
                for kt in range(nkt):
                    mu = cnt[0]
                    mb_, mbt = ps[2 + mu % 2], ("bank", 2 + mu % 2)
                    P.op("pe", lambda e, kt=kt, mb_=mb_: e.matmul(mb_[:, 0:128], lhsT=E_all[:, kt % 64, :], rhs=selT[:, kt // 64, :], start=True, stop=True),
                         reads=["E_all", "selT"], writes=[mbt])
                    if kt >= 8 * j:
                        ms_ = msb[mu % 2]
                        P.op("dve", lambda e, kt=kt, mb_=mb_, ms_=ms_: e.tensor_tensor(out=ms_[:], in0=mb_[:, 0:128], in1=cmask[:, kt - 8 * j, :], op=ALU.mult),
                             reads=[mbt, "cmask"], writes=[("msb", mu % 2)])
                        mk, mr = ms_[:], [("msb", mu % 2)]
                    else:
                        mk, mr = mb_[:, 0:128], [mbt]
                    unit(j, h, ksT[64 * h:64 * h + 64, kt * 128:(kt + 1) * 128], ks_reads, vsA[:, kt, VE * h:VE * h + 65], vs_reads,
                         mk, mr, kt == 0, kt == nkt - 1)
                evac(1)
                for kw_ in range(lo, 12):
                    unit(j, h, kwT[wb][64 * h:64 * h + 64, kw_, :], [("kwT", wb)], vwB[wb][:, kw_, VE * h:VE * h + 65], [("vwB", wb)],
                         wmask[:, kw_, :], ["wmask"], kw_ == lo, kw_ == 11)
                evac(2)
                for i in range(4):
                    hh = 4 * h + i
                    for br in range(3):
                        P.op("pe", lambda e, i=i, br=br: e.transpose(out=ps[5][:, br * 128:(br + 1) * 128], in_=obr[br][:, i * 128:(i + 1) * 128],
                                                                     identity=k.identF[:]),
                             reads=[("obr", br), "identF"], writes=[("bank", 5)])
                    tp = ps[5][:, 0:384].rearrange("p (b e) -> p b e", b=3)
                    P.op("dve", lambda e, tp=tp: e.tensor_scalar(out=small[:, 8:11], in0=tp[:, :, 64], scalar1=1e-30, scalar2=None, op0=ALU.max),
                         reads=[("bank", 5)], writes=["small2"])
                    P.op("dve", lambda e: e.reciprocal(out=small[:, 8:11], in_=small[:, 8:11]), reads=["small2"], writes=["small2"])
                    P.op("dve", lambda e, hh=hh: e.tensor_tensor(out=small[:, 12:15], in0=small[:, 8:11], in1=k.gates[:, j, hh * 3:hh * 3 + 3], op=ALU.mult),
                         reads=["small2", ("gates", j)], writes=["small3"])
                    P.op("dve", lambda e, hh=hh, tp=tp: e.tensor_scalar(out=oacc[:, hh, :], in0=tp[:, 0, 0:64], scalar1=small[:, 12:13], scalar2=None, op0=ALU.mult),
                         reads=[("bank", 5), "small3"], writes=[("oacc", hh)])
                    for br in (1, 2):
                        P.op("dve", lambda e, hh=hh, tp=tp, br=br: e.scalar_tensor_tensor(out=oacc[:, hh, :], in0=tp[:, br, 0:64], scalar=small[:, 12 + br:13 + br],
                                                                                          in1=oacc[:, hh, :], op0=ALU.mult, op1=ALU.add),
                             reads=[("bank", 5), "small3", ("oacc", hh)], writes=[("oacc", hh)])
            oreads = [("oacc", hh) for hh in range(8)]
            P.op("dve", lambda e: e.tensor_tensor(out=sqt[:], in0=oacc[:], in1=oacc[:], op=ALU.mult), reads=oreads, writes=["sqt"])
            P.op("dve", lambda e: e.reduce_sum(out=small[:, 16:17], in_=sqt[:].rearrange("p a d -> p (a d)"), axis=AX.X), reads=["sqt"], writes=["small4"])
            P.op("act", lambda e: e.activation(out=small[:, 16:17], in_=small[:, 16:17], func=AF.Sqrt, scale=1.0 / 512, bias=EPS),
                 reads=["small4"], writes=["small4"])
            P.op("dve", lambda e: e.reciprocal(out=small[:, 16:17], in_=small[:, 16:17]), reads=["small4"], writes=["small4"])
            P.op("dve", lambda e: e.scalar_tensor_tensor(out=mixtok[:], in0=oacc[:].rearrange("p a d -> p (a d)"), scalar=small[:, 16:17], in1=agB[:],
                                                         op0=ALU.mult, op1=ALU.mult), reads=oreads + ["small4", "agB"], writes=["mixtok"])
            for ft in range(4):
                P.op("pe", lambda e, ft=ft: e.transpose(out=ps[6][:, ft * 128:(ft + 1) * 128], in_=mixtok[:, ft * 128:(ft + 1) * 128], identity=k.identF[:]),
                     reads=["mixtok", "identF"], writes=[("bank", 6)])
            P.op("act", lambda e: e.copy(out=k.mixT[:, 0:4, j * 128:(j + 1) * 128], in_=ps[6][:].rearrange("p (f q) -> p f q", f=4)),
                 reads=[("bank", 6)], writes=[("mixT", "a", j)])

        for j in range(NJ):
            do_seg(j)
        if k.dbg is not None and k.stop_after == 3:
            P.dma("pool", k.dbg[:, 0:4 * TOK].rearrange("p (f t) -> p f t", f=4)[:, :, 0:NJ * 128], k.mixT[:, 0:4, 0:NJ * 128],
                  reads=[("mixT", "a", j) for j in range(NJ)], key="dbg")
        P.emit()


def phase_ssm(k):
    nc, P, I = k.nc, k.P, k.I
    with ExitStack() as st:
        sb = lambda name, shape, dt: st.enter_context(nc.sbuf_tensor("sb_" + name, shape, dt))
        ps = k.psum
        uid = [0]

        cur = [st]

        def T(shape, dt=F32):
            uid[0] += 1
            return cur[0].enter_context(nc.sbuf_tensor("sb_ssm%d" % uid[0], shape, dt))

        def dv(fn, reads, writes):
            P.op("dve", fn, reads=reads, writes=writes)

        def tt(out, a, b, op, rd, wr):
            dv(lambda e: e.tensor_tensor(out=out, in0=a, in1=b, op=op), rd, wr)

        prm = {}
        for n, shp in [("lamre", [128, 16]), ("lamim", [128, 16]), ("lstep", [128, 16]), ("bre", [128, 16, 16]), ("bim", [128, 16, 16]),
                       ("cre", [128, 16, 16]), ("cim", [128, 16, 16]), ("dskipT", [128, 4]), ("b_gluT", [128, 4]), ("sgT", [128, 4]),
                       ("onehot", [128, 8])]:
            prm[n] = T(shp)
            ld(k, prm[n][:] if len(shp) == 2 else prm[n][:].rearrange("p a b -> p (a b)"), I[n], n, ("p", n))
        wglu = T([128, 4, 512], BF16)
        P.dma("pool", wglu[:], I["w_glu"].rearrange("p (t o) -> p t o", t=4), writes=["wglu"], key="wglu")
        lr, li, ls = prm["lamre"], prm["lamim"], prm["lstep"]
        W = T([128, 24, 16])
        w = lambda i: W[:, i, :]
        step, mag, cs, sn, are, aim = (w(i) for i in range(6))
        P.op("act", lambda e: e.activation(out=step, in_=ls[:], func=AF.Exp), reads=["lstep"], writes=["step"])
        tt(w(6), lr[:], step, ALU.mult, ["lamre", "step"], ["w6"])
        P.op("act", lambda e: e.activation(out=mag, in_=w(6), func=AF.Exp), reads=["w6"], writes=["mag"])
        tt(w(7), li[:], step, ALU.mult, ["lamim", "step"], ["ang"])
        halfpi = T([128, 1])
        dv(lambda e: e.memset(halfpi[:], float(np.pi / 2)), [], ["halfpi"])
        P.op("act", lambda e: e.activation(out=sn, in_=w(7), func=AF.Sin, scale=1.0 / 64), reads=["ang"], writes=["sn"])
        P.op("act", lambda e: e.activation(out=cs, in_=w(7), func=AF.Sin, scale=1.0 / 64, bias=halfpi[:]), reads=["ang", "halfpi"], writes=["cs"])
        for it in range(6):
            tt(w(8), cs, cs, ALU.mult, ["cs"], ["w8"])
            tt(w(9), sn, sn, ALU.mult, ["sn"], ["w9"])
            tt(w(10), cs, sn, ALU.mult, ["cs", "sn"], ["w10"])
            tt(cs, w(8), w(9), ALU.subtract, ["w8", "w9"], ["cs"])
            tt(sn, w(10), w(10), ALU.add, ["w10"], ["sn"])
        tt(are, mag, cs, ALU.mult, ["mag", "cs"], ["are"])
        tt(aim, mag, sn, ALU.mult, ["mag", "sn"], ["aim"])
        tt(w(8), lr[:], lr[:], ALU.mult, ["lamre"], ["w8"])
        tt(w(9), li[:], li[:], ALU.mult, ["lamim"], ["w9"])
        tt(w(8), w(8), w(9), ALU.add, ["w8", "w9"], ["w8"])
        dv(lambda e: e.reciprocal(out=w(8), in_=w(8)), ["w8"], ["w8"])
        dv(lambda e: e.tensor_scalar(out=w(9), in0=are, scalar1=-1.0, scalar2=None, op0=ALU.add), ["are"], ["nre"])
        fre, fim = w(11), w(12)
        tt(w(10), w(9), lr[:], ALU.mult, ["nre", "lamre"], ["w10"])
        tt(w(13), aim, li[:], ALU.mult, ["aim", "lamim"], ["w13"])
        tt(w(10), w(10), w(13), ALU.add, ["w10", "w13"], ["w10"])
        tt(fre, w(10), w(8), ALU.mult, ["w10", "w8"], ["fre"])
        tt(w(10), aim, lr[:], ALU.mult, ["aim", "lamre"], ["w10"])
        tt(w(13), w(9), li[:], ALU.mult, ["nre", "lamim"], ["w13"])
        tt(w(10), w(10), w(13), ALU.subtract, ["w10", "w13"], ["w10"])
        tt(fim, w(10), w(8), ALU.mult, ["w10", "w8"], ["fim"])
        bc16 = lambda ap: ap.unsqueeze(2).to_broadcast([128, 16, 16])
        bbre, bbim, t3a, t3b = T([128, 16, 16]), T([128, 16, 16]), T([128, 16, 16]), T([128, 16, 16])

        def cmul3(ore, oim, xre, xim, yre, yim, rd, wr):
            tt(t3a[:], yre, bc16(xre), ALU.mult, rd, ["t3a"])
            tt(t3b[:], yim, bc16(xim), ALU.mult, rd, ["t3b"])
            tt(ore, t3a[:], t3b[:], ALU.subtract, ["t3a", "t3b"], [wr + "re"])
            tt(t3a[:], yim, bc16(xre), ALU.mult, rd, ["t3a"])
            tt(t3b[:], yre, bc16(xim), ALU.mult, rd, ["t3b"])
            tt(oim, t3a[:], t3b[:], ALU.add, ["t3a", "t3b"], [wr + "im"])

        cmul3(bbre[:], bbim[:], fre, fim, prm["bre"][:], prm["bim"][:], ["fre", "fim", "bre", "bim"], "bb")
        Apr, Api = T([128, 9, 16]), T([128, 9, 16])
        dv(lambda e: e.memset(Apr[:, 0, :], 1.0), [], [("Ap", 0)])
        dv(lambda e: e.memset(Api[:, 0, :], 0.0), [], [("Ap", 0)])

        def cmul2(ore, oim, xre, xim, yre, yim, rd, wr):
            tt(w(14), xre, yre, ALU.mult, rd, ["w14"])
            tt(w(15), xim, yim, ALU.mult, rd, ["w15"])
            tt(w(16), xre, yim, ALU.mult, rd, ["w16"])
            tt(w(17), xim, yre, ALU.mult, rd, ["w17"])
            tt(ore, w(14), w(15), ALU.subtract, ["w14", "w15"], wr)
            tt(oim, w(16), w(17), ALU.add, ["w16", "w17"], wr)

        for tau in range(1, 9):
            cmul2(Apr[:, tau, :], Api[:, tau, :], Apr[:, tau - 1, :], Api[:, tau - 1, :], are, aim, [("Ap", tau - 1), "are", "aim"], [("Ap", tau)])
        Hp = T([128, 12, 16])
        h = lambda i: Hp[:, i, :]
        dv(lambda e: e.tensor_copy(out=h(0), in_=Apr[:, 8, :]), [("Ap", 8)], ["a128"])
        dv(lambda e: e.tensor_copy(out=h(1), in_=Api[:, 8, :]), [("Ap", 8)], ["a128"])
        for it in range(4):
            cmul2(h(2), h(3), h(0), h(1), h(0), h(1), ["a128"], ["a128t"])
            dv(lambda e: e.tensor_copy(out=h(0), in_=h(2)), ["a128t"], ["a128"])
            dv(lambda e: e.tensor_copy(out=h(1), in_=h(3)), ["a128t"], ["a128"])
        P128r, P128i = T([128, 9, 16]), T([128, 9, 16])
        dv(lambda e: e.memset(P128r[:, 0, :], 1.0), [], [("P128", 0)])
        dv(lambda e: e.memset(P128i[:, 0, :], 0.0), [], [("P128", 0)])
        for c in range(1, 9):
            cmul2(P128r[:, c, :], P128i[:, c, :], P128r[:, c - 1, :], P128i[:, c - 1, :], h(0), h(1), [("P128", c - 1), "a128"], [("P128", c)])
        gre, gim = T([128, 16, 16]), T([128, 16, 16])
        Kmat = T([128, 4, 8, 32], BF16)
        S1, S2 = T([128, 16, 16]), T([128, 16, 16])
        Lst = T([128, 2, 16, 16])
        Xs = [T([128, 16, 16, 17]), T([128, 16, 16, 17])]
        stBc = ExitStack()
        cur[0] = stBc
        Bc = [T([128, 16, 256]), T([128, 16, 256])]

        def put(dst, src, rd, wr, neg=False):
            for hf in range(2):
                if neg:
                    dv(lambda e, hf=hf: e.tensor_scalar(out=dst[hf * 64:(hf + 1) * 64, :, hf * 16:(hf + 1) * 16], in0=src[hf * 64:(hf + 1) * 64, :, :],
                                                        scalar1=-1.0, scalar2=None, op0=ALU.mult), rd, wr)
                else:
                    dv(lambda e, hf=hf: e.tensor_copy(out=dst[hf * 64:(hf + 1) * 64, :, hf * 16:(hf + 1) * 16], in_=src[hf * 64:(hf + 1) * 64, :, :]), rd, wr)

        def make_CA(taus):
            CAr, CAi = T([128, 9, 16, 32]), T([128, 9, 16, 32])
            for tns, nm in [(CAr, "CAr"), (CAi, "CAi")]:
                P.op("pool", lambda e, tns=tns: e.memset(tns[:], 0.0), writes=[nm])
            for tau in taus:
                cmul3(gre[:], gim[:], Apr[:, tau, :], Api[:, tau, :], prm["cre"][:], prm["cim"][:], [("Ap", tau), "cre", "cim"], "g")
                put(CAr[:, tau, :, :], gre, ["gre"], ["CAr"])
                put(CAi[:, tau, :, :], gim, ["gim"], ["CAi"], neg=True)
            return CAr, CAi

        stA = ExitStack()
        cur[0] = stA
        CA1r, CA1i = T([128, 16, 32]), T([128, 16, 32])
        for tns, nm in [(CA1r, "CA1r"), (CA1i, "CA1i")]:
            P.op("pool", lambda e, tns=tns: e.memset(tns[:], 0.0), writes=[nm])
        BBr, BBi = T([128, 16, 32]), T([128, 16, 32])
        Mr, Mi = T([128, 16, 32]), T([128, 16, 32])
        for tns, nm in [(BBr, "BBr"), (BBi, "BBi"), (Mr, "Mr"), (Mi, "Mi")]:
            P.op("pool", lambda e, tns=tns: e.memset(tns[:], 0.0), writes=[nm])
        put(BBr, bbre, ["bbre"], ["BBr"])
        put(BBi, bbim, ["bbim"], ["BBi"])
        W1T = T([128, 4, 8, 2, 128], BF16)
        for r in range(8):
            cmul3(gre[:], gim[:], Apr[:, 7 - r, :], Api[:, 7 - r, :], bbre[:], bbim[:], [("Ap", 7 - r), "bbre", "bbim"], "g")
            put(Mr, gre, ["gre"], ["Mr"])
            put(Mi, gim, ["gim"], ["Mi"])
            for ri, (M_, mn) in enumerate([(Mr, "Mr"), (Mi, "Mi")]):
                for gh in range(4):
                    bnk = (r * 8 + ri * 4 + gh) % 4
                    P.op("pe", lambda e, M_=M_, gh=gh, bnk=bnk: e.transpose(out=ps[bnk][:, 0:128], in_=M_[:, 4 * gh:4 * gh + 4, :].rearrange("p a b -> p (a b)"),
                                                                          identity=k.identF[:]), reads=[mn, "identF"], writes=[("bank", bnk)])
                    P.op("act", lambda e, gh=gh, r=r, ri=ri, bnk=bnk: e.copy(out=W1T[:, gh, r, ri, :], in_=ps[bnk][:, 0:128]),
                         reads=[("bank", bnk)], writes=["W1T"])
        for tau in range(8):
            cmul3(gre[:], gim[:], Apr[:, tau, :], Api[:, tau, :], prm["cre"][:], prm["cim"][:], [("Ap", tau), "cre", "cim"], "g")
            put(CA1r, gre, ["gre"], ["CA1r"])
            put(CA1i, gim, ["gim"], ["CA1i"], neg=True)
            for gh in range(4):
                for gq in range(4):
                    gp = 4 * gh + gq
                    for ri, (B_, C_) in enumerate([(BBr, CA1r), (BBi, CA1i)]):
                        P.op("pe", lambda e, gq=gq, gp=gp, gh=gh, tau=tau, B_=B_, C_=C_, ri=ri: e.matmul(
                            ps[4][32 * gq:32 * gq + 32, gh * 32:(gh + 1) * 32], lhsT=B_[:, gp, :], rhs=C_[:, gp, :],
                            start=(ri == 0), stop=(ri == 1), tile_position=(0, 32 * gq)), reads=["BBr", "BBi", "CA1r", "CA1i"], writes=[("bank", 4)])
            P.op("act", lambda e, tau=tau: e.copy(out=Kmat[:, :, tau, :], in_=ps[4][:, 0:128].rearrange("p (a b) -> p a b", a=4)),
                 reads=[("bank", 4)], writes=["Kmat"])
        zs_reads = [("zsT", si, tg) for si in range(4) for tg in range(4)]
        for gp in range(16):
            gh, gq = gp // 4, gp % 4
            for ri in range(2):
                bnk = (gp * 2 + ri) % 4
                for r in range(8):
                    P.op("pe", lambda e, gh=gh, gq=gq, ri=ri, r=r, bnk=bnk: e.matmul(
                        ps[bnk][:, 0:256], lhsT=W1T[32 * gq:32 * gq + 32, gh, r, ri, :], rhs=k.zsT[32 * gq:32 * gq + 32, gh, r:TOK:8],
                        start=(r == 0), stop=(r == 7), tile_position=(32 * gq, 0)), reads=["W1T"] + zs_reads, writes=[("bank", bnk)])
                P.op("act", lambda e, gp=gp, ri=ri, bnk=bnk: e.copy(out=Bc[ri][:, gp, :], in_=ps[bnk][:, 0:256]),
                     reads=[("bank", bnk)], writes=[("Bc", ri)])
        a8r, a8i = bc16(Apr[:, 8, :]), bc16(Api[:, 8, :])

        def scan(prev, nxt, ptok, ntok):
            for jp in range(16):
                xr, xi = prev(0, jp), prev(1, jp)
                nr, ni = nxt(0, jp), nxt(1, jp)
                br = Bc[0][:].rearrange("p g (s j) -> p g s j", j=16)[:, :, :, jp]
                bi = Bc[1][:].rearrange("p g (s j) -> p g s j", j=16)[:, :, :, jp]
                rd = [ptok(jp), ("Ap", 8)]
                tt(S1[:], xr, a8r, ALU.mult, rd, ["S1"])
                tt(S2[:], xi, a8i, ALU.mult, rd, ["S2"])
                tt(S1[:], S1[:], S2[:], ALU.subtract, ["S1", "S2"], ["S1"])
                tt(nr, S1[:], br, ALU.add, ["S1", ("Bc", 0)], [ntok(jp)])
                tt(S1[:], xi, a8r, ALU.mult, rd, ["S1"])
                tt(S2[:], xr, a8i, ALU.mult, rd, ["S2"])
                tt(S1[:], S1[:], S2[:], ALU.add, ["S1", "S2"], ["S1"])
                tt(ni, S1[:], bi, ALU.add, ["S1", ("Bc", 1)], [ntok(jp)])

        XP = [[T([128, 16, 16]), T([128, 16, 16])] for _ in range(2)]
        for ri in range(2):
            P.op("pool", lambda e, ri=ri: e.memset(XP[0][ri][:], 0.0), writes=[("XP", 0)])
        scan(lambda ri, jp: XP[jp % 2][ri][:], lambda ri, jp: XP[(jp + 1) % 2][ri][:], lambda jp: ("XP", jp % 2), lambda jp: ("XP", (jp + 1) % 2))
        for ri in range(2):
            dv(lambda e, ri=ri: e.tensor_copy(out=Lst[:, ri, :, :], in_=XP[0][ri][:]), [("XP", 0)], ["Lst"])
        P.dma("sp", k.lin, Lst[:].rearrange("p a b c -> p (a b c)"), reads=["Lst"], writes=["lin"], key="lin")
        P.emit()
        stA.close()
        stB = ExitStack()
        cur[0] = stB
        if not os.environ.get("SKIP_AG"):
            P.op("pool", lambda e: e.collective_compute("AllGather", ALU.bypass, replica_groups=[list(range(NCORE))],
                                                        ins=[k.lin.opt()], outs=[k.lall.opt()]), writes=["lall"], dma="cc2", inc=1)
        Lall = T([128, 8, 2, 16, 16])
        P.dma("sp", Lall[:].rearrange("p c a b d -> p c (a b d)"), k.lall.rearrange("(c p) w -> p c w", p=128), reads=["lall"], writes=["Lall"], key="Lall")
        Q = [T([128, 8, 16, 16]), T([128, 8, 16, 16])]
        p1r, p1i = bc16(P128r[:, 1, :]), bc16(P128i[:, 1, :])
        for ri in range(2):
            dv(lambda e, ri=ri: e.tensor_copy(out=Q[ri][:, 0, :, :], in_=Lall[:, 0, ri, :, :]), ["Lall"], [("Q", 0)])
        for c in range(1, 8):
            rd = [("Q", c - 1), ("P128", 1)]
            tt(S1[:], Q[0][:, c - 1], p1r, ALU.mult, rd, ["S1"])
            tt(S2[:], Q[1][:, c - 1], p1i, ALU.mult, rd, ["S2"])
            tt(S1[:], S1[:], S2[:], ALU.subtract, ["S1", "S2"], ["S1"])
            tt(Q[0][:, c], S1[:], Lall[:, c, 0, :, :], ALU.add, ["S1", "Lall"], [("Q", c)])
            tt(S1[:], Q[1][:, c - 1], p1r, ALU.mult, rd, ["S1"])
            tt(S2[:], Q[0][:, c - 1], p1i, ALU.mult, rd, ["S2"])
            tt(S1[:], S1[:], S2[:], ALU.add, ["S1", "S2"], ["S1"])
            tt(Q[1][:, c], S1[:], Lall[:, c, 1, :, :], ALU.add, ["S1", "Lall"], [("Q", c)])
        Rp = [T([128, 16, 17]), T([128, 16, 17])]
        for ri in range(2):
            dv(lambda e, ri=ri: e.memset(Rp[ri][:], 0.0), [], [("Rp", 0)])
        for j in range(15):
            rd = [("Rp", j), ("P128", 8)]
            tt(h(4), Rp[0][:, :, j], P128r[:, 8, :], ALU.mult, rd, ["h4"])
            tt(h(5), Rp[1][:, :, j], P128i[:, 8, :], ALU.mult, rd, ["h5"])
            tt(h(4), h(4), h(5), ALU.subtract, ["h4", "h5"], ["h4"])
            tt(Rp[0][:, :, j + 1], h(4), Q[0][:, 7, :, j], ALU.add, ["h4", ("Q", 7)], [("Rp", j + 1)])
            tt(h(4), Rp[1][:, :, j], P128r[:, 8, :], ALU.mult, rd, ["h4"])
            tt(h(5), Rp[0][:, :, j], P128i[:, 8, :], ALU.mult, rd, ["h5"])
            tt(h(4), h(4), h(5), ALU.add, ["h4", "h5"], ["h4"])
            tt(Rp[1][:, :, j + 1], h(4), Q[1][:, 7, :, j], ALU.add, ["h4", ("Q", 7)], [("Rp", j + 1)])
        oh = prm["onehot"]
        rp_reads = [("Rp", j) for j in range(16)]
        for c in range(8):
            rd = rp_reads + [("P128", c)]
            pr, pi = bc16(P128r[:, c, :]), bc16(P128i[:, c, :])
            for ri in range(2):
                a, b = (0, 1) if ri == 0 else (1, 0)
                tt(S1[:], Rp[a][:, :, 0:16], pr, ALU.mult, rd, ["S1"])
                tt(S2[:], Rp[b][:, :, 0:16], pi, ALU.mult, rd, ["S2"])
                tt(S1[:], S1[:], S2[:], ALU.subtract if ri == 0 else ALU.add, ["S1", "S2"], ["S1"])
                if c > 0:
                    tt(S1[:], S1[:], Q[ri][:, c - 1], ALU.add, ["S1", ("Q", c - 1)], ["S1"])
                if c == 0:
                    dv(lambda e, ri=ri, c=c: e.tensor_scalar(out=Xs[ri][:, :, :, 0], in0=S1[:], scalar1=oh[:, c:c + 1], scalar2=None, op0=ALU.mult),
                       ["S1", "onehot"], [("Xs", 0)])
                else:
                    dv(lambda e, ri=ri, c=c: e.scalar_tensor_tensor(out=Xs[ri][:, :, :, 0], in0=S1[:], scalar=oh[:, c:c + 1], in1=Xs[ri][:, :, :, 0],
                                                                    op0=ALU.mult, op1=ALU.add), ["S1", "onehot", ("Xs", 0)], [("Xs", 0)])
        scan(lambda ri, jp: Xs[ri][:, :, :, jp], lambda ri, jp: Xs[ri][:, :, :, jp + 1], lambda jp: ("Xs", jp), lambda jp: ("Xs", jp + 1))
        P.emit()
        stB.close()
        stBc.close()
        stC = ExitStack()
        cur[0] = stC
        CAr, CAi = make_CA(range(1, 9))
        ylin = T([128, 4, 512])
        ygb = T([128, 4, 512], BF16)
        sg = T([128, 512])
        yss = T([128, 4, 512])
        ysq = T([128, 4, 512], BF16)
        rst = T([128, 512])
        xs_reads = [("Xs", jp) for jp in range(17)]

        def do_tg(tg):
            ts = slice(tg * 512, (tg + 1) * 512)
            for gh in range(4):
                bnk = gh % 2
                bk = ps[bnk]
                for gq in range(4):
                    gp = 4 * gh + gq
                    for r in range(8):
                        o_ = bk[32 * gq:32 * gq + 32, r:512:8]
                        n_mm = 2 + r + 1
                        i_mm = 0
                        for ri, C_ in enumerate([CAr, CAi]):
                            P.op("pe", lambda e, o_=o_, C_=C_, ri=ri, r=r, gp=gp, i_mm=i_mm, n_mm=n_mm: e.matmul(
                                o_, lhsT=C_[:, r + 1, gp, :], rhs=Xs[ri][:, gp, 4 * tg:4 * tg + 4, 0:16], start=(i_mm == 0), stop=(i_mm == n_mm - 1),
                                skip_group_check=True, tile_position=(0, 32 * (gp % 4))), reads=["CAr", "CAi"] + xs_reads, writes=[("bank", bnk)])
                            i_mm += 1
                        for r2 in range(r + 1):
                            P.op("pe", lambda e, o_=o_, r=r, r2=r2, gh=gh, gq=gq, i_mm=i_mm, n_mm=n_mm: e.matmul(
                                o_, lhsT=Kmat[32 * gq:32 * gq + 32, gh, r - r2, :], rhs=k.zsT[32 * gq:32 * gq + 32, gh, tg * 512 + r2:(tg + 1) * 512:8],
                                start=False, stop=(i_mm == n_mm - 1), skip_group_check=True, tile_position=(32 * gq, 32 * gq)), reads=["Kmat"] + zs_reads, writes=[("bank", bnk)])
                            i_mm += 1
                dv(lambda e, gh=gh, bk=bk: e.scalar_tensor_tensor(out=ylin[:, gh, :], in0=k.zsT[:, gh, ts], scalar=prm["dskipT"][:, gh:gh + 1], in1=bk[:],
                                                                  op0=ALU.mult, op1=ALU.add), [("bank", bnk), "dskipT"] + zs_reads, [("ylin", gh)])
                P.op("act", lambda e, gh=gh: e.activation(out=ylin[:, gh, :], in_=ylin[:, gh, :], func=AF.Gelu_apprx_tanh), reads=[("ylin", gh)], writes=[("ylin", gh)])
                dv(lambda e, gh=gh: e.tensor_copy(out=ygb[:, gh, :], in_=ylin[:, gh, :]), [("ylin", gh)], [("ygb", gh)])
            for ot in range(4):
                bnk = 2 + ot % 2
                for kt in range(4):
                    P.op("pe", lambda e, ot=ot, kt=kt, bnk=bnk: e.matmul(ps[bnk][:], lhsT=wglu[:, kt, ot * 128:(ot + 1) * 128], rhs=ygb[:, kt, :],
                                                                         start=(kt == 0), stop=(kt == 3)), reads=["wglu", ("ygb", kt)], writes=[("bank", bnk)])
                P.op("act", lambda e, ot=ot, bnk=bnk: e.activation(out=sg[:], in_=ps[bnk][:], func=AF.Sigmoid, bias=prm["b_gluT"][:, ot:ot + 1]),
                     reads=[("bank", bnk), "b_gluT"], writes=["sg"])
                tt(yss[:, ot, :], ylin[:, ot, :], sg[:], ALU.mult, [("ylin", ot), "sg"], [("yss", ot)])
                P.op("act", lambda e, ot=ot: e.activation(out=ysq[:, ot, :], in_=yss[:, ot, :], func=AF.Square), reads=[("yss", ot)], writes=[("ysq", ot)])
            for ot in range(4):
                P.op("pe", lambda e, ot=ot: e.matmul(ps[6][:], lhsT=k.onesB[:], rhs=ysq[:, ot, :], start=(ot == 0), stop=(ot == 3)),
                     reads=["onesB", ("ysq", ot)], writes=[("bank", 6)])
            P.op("act", lambda e: e.activation(out=rst[:], in_=ps[6][:], func=AF.Sqrt, scale=1.0 / 512, bias=EPS), reads=[("bank", 6)], writes=["rst"])
            dv(lambda e: e.reciprocal(out=rst[:], in_=rst[:]), ["rst"], ["rst"])
            for ot in range(4):
                tt(yss[:, ot, :], yss[:, ot, :], rst[:], ALU.mult, [("yss", ot), "rst"], [("yss", ot)])
                P.op("act", lambda e, ot=ot: e.activation(out=k.mixT[:, 4 + ot, ts], in_=yss[:, ot, :], func=AF.Copy, scale=prm["sgT"][:, ot:ot + 1]),
                     reads=[("yss", ot), "sgT"], writes=[("mixT", "s", ot, tg)])

        for tg in range(4):
            do_tg(tg)
        if k.dbg is not None and k.stop_after == 4:
            P.dma("pool", k.dbg[:, 0:4 * TOK].rearrange("p (f t) -> p f t", f=4), k.mixT[:, 4:8, :],
                  reads=[("mixT", "s", ot, tg) for ot in range(4) for tg in range(4)], key="dbg")
        P.emit()
        stC.close()


def phase4(k, st1):
    nc, P, I = k.nc, k.P, k.I
    ps = k.psum
    with ExitStack() as stH:
        h2T = k.qz
        with ExitStack() as st:
            sb = lambda name, shape, dt: st.enter_context(nc.sbuf_tensor("sb_" + name, shape, dt))
            wo = sb("wo", [128, 8, D], BF16)
            xtok = sb("xtok4", [128, 4, D], F32)
            xT = sb("xT4", [128, 8, 512], F32)
            x1T = sb("x1T", [128, 8, 512], F32)
            sq = sb("sq4", [128, 8, 512], BF16)
            rstd = sb("rstd4", [128, 512], F32)
            ntmp = sb("ntmp4", [128, 2, 512], F32)
            hst = sb("hst", [128, 8, NSEG, 2], BF16)
            for kt in range(8):
                P.dma("pool", wo[:, kt, :], I["w_o"][:, kt * 1024:(kt + 1) * 1024], writes=[("wo", kt)], key=("wo", kt))
            bi = [0]

            def bank():
                b = bi[0] % 6
                bi[0] += 1
                return ps[b], ("bank", b)

            def do_tg(tg):
                ts = slice(tg * 512, (tg + 1) * 512)
                P.dma("sp", xtok[:], I["x_c"][:, tg * 4096:(tg + 1) * 4096].rearrange("p (s d) -> p s d", s=4), writes=["xtok"], key="xtok")
                for ft in range(8):
                    b, bt = bank()
                    for s_ in range(4):
                        P.op("pe", lambda e, b=b, s_=s_, ft=ft: e.transpose(out=b[:, s_ * 128:(s_ + 1) * 128], in_=xtok[:, s_, ft * 128:(ft + 1) * 128], identity=k.identF[:]),
                             reads=["xtok", "identF"], writes=[bt])
                    P.op("act", lambda e, b=b, ft=ft: e.copy(out=xT[:, ft, :], in_=b[:]), reads=[bt], writes=[("xT", ft)])
                for ot in range(8):
                    b, bt = bank()
                    for kt in range(8):
                        P.op("pe", lambda e, b=b, ot=ot, kt=kt: e.matmul(b[:], lhsT=wo[:, kt, ot * 128:(ot + 1) * 128], rhs=k.mixT[:, kt, ts], start=(kt == 0), stop=(kt == 7)),
                             reads=[("wo", kt)], writes=[bt])
                    P.op("dve", lambda e, b=b, ot=ot: e.scalar_tensor_tensor(out=x1T[:, ot, :], in0=b[:], scalar=k.modT[:, 16 + ot:17 + ot], in1=xT[:, ot, :],
                                                                            op0=ALU.mult, op1=ALU.add), reads=[bt, ("xT", ot)], writes=[("x1", "src", ot)])
                P.dma("sp", k.x1d[:, :, ts], x1T[:], reads=[("x1", "src", ot) for ot in range(8)], writes=[("x1d", tg)], key="x1d")
                rmsnorm_T(k, ("x1",), [x1T[:, ft, :] for ft in range(8)], [h2T[:, ft, ts] for ft in range(8)], k.g2s, k.modT[:, 24:32], sq, rstd, ntmp, ps[7])
                for ft in range(8):
                    P.op("pool", lambda e, ft=ft: e.tensor_copy(out=hst[:, ft, 4 * tg:4 * tg + 4, :],
                                                               in_=h2T[:, ft, ts].rearrange("p (s r) -> p s r", s=4)[:, :, 126:128]),
                         reads=[("x1", "dst", ft)], writes=["hst"])

            for tg in range(4):
                do_tg(tg)
            P.dma("sp", k.hin, hst[:].rearrange("p a b c -> p (a b c)"), reads=["hst"], writes=["hin"], key="hin")
            P.emit()
        k.mixT = None
        st1.close()
        gT = stH.enter_context(nc.sbuf_tensor("sb_gT", [128, 22, TOK], BF16))
        with ExitStack() as st:
            sb = lambda name, shape, dt: st.enter_context(nc.sbuf_tensor("sb_" + name, shape, dt))
            if not os.environ.get("SKIP_AG"):
                P.op("pool", lambda e: e.collective_compute("AllGather", ALU.bypass, replica_groups=[list(range(NCORE))],
                                                            ins=[k.hin.opt()], outs=[k.hall.opt()]), writes=["hall"], dma="cc3", inc=1)
            Hall = sb("Hall", [128, 8, 8, NSEG, 2], BF16)
            hself = sb("hself", [128, 8, NSEG, 2], F32)
            hsel = sb("hsel", [128, 8, NSEG, 2], BF16)
            oh = sb("oh4", [128, 8], F32)
            cw = sb("cw", [128, 3, 44], F32)
            cb = sb("cb", [128, 44], F32)
            wu = [sb("wu%d" % i, [128, 8, 2, 128], BF16) for i in range(2)]
            ue = [sb("ue%d" % i, [128, 4, 130], F32) for i in range(2)]
            acc = [sb("cacc%d" % i, [128, 4, 128], F32) for i in range(2)]
            uhs = sb("uhs", [128, 2, NSEG, 2], F32)
            sa = sb("sa", [128, 512], F32)
            P.dma("sp", Hall[:].rearrange("p c a b d -> p c (a b d)"), k.hall.rearrange("(c p) w -> p c w", p=128), reads=["hall"], writes=["Hall"], key="Hall")
            ld(k, oh[:], I["onehot"], "oh4", "oh4")
            ld(k, cw[:], I["conv_wT"].rearrange("p (a b) -> p a b", a=3), "cw", "cw")
            ld(k, cb[:], I["conv_bT"], "cb", "cb")
            P.op("dve", lambda e: e.memset(hself[:], 0.0), writes=["hself"])
            for cp in range(7):
                P.op("dve", lambda e, cp=cp: e.scalar_tensor_tensor(out=hself[:], in0=Hall[:, cp], scalar=oh[:, cp + 1:cp + 2], in1=hself[:], op0=ALU.mult, op1=ALU.add),
                     reads=["Hall", "oh4", "hself"], writes=["hself"])
            P.op("dve", lambda e: e.scalar_tensor_tensor(out=hself[:, :, 1:NSEG, :], in0=Hall[:, 7, :, 0:NSEG - 1, :], scalar=oh[:, 0:1], in1=hself[:, :, 1:NSEG, :],
                                                         op0=ALU.mult, op1=ALU.add), reads=["Hall", "oh4", "hself"], writes=["hself"])
            P.op("dve", lambda e: e.tensor_copy(out=hsel[:], in_=hself[:]), reads=["hself"], writes=["hsel"])
            wuv = I["w_up"].rearrange("p (t c) -> p t c", t=8)

            def do_ct(ct):
                wb = ct % 2
                for av in range(2):
                    c0 = av * 2816 + ct * 128
                    P.dma("pool", wu[wb][:, :, av, :], wuv[:, :, c0:c0 + 128], writes=[("wu", wb, av)], key=("wu", wb, av))
                for av in range(2):
                    for kt in range(8):
                        P.op("pe", lambda e, av=av, kt=kt: e.matmul(ps[6][:, av * 32:av * 32 + 32], lhsT=wu[wb][:, kt, av, :], rhs=hsel[:, kt, :, :].rearrange("p a b -> p (a b)"),
                                                                   start=(kt == 0 and av == 0), stop=(kt == 7), skip_group_check=True),
                             reads=[("wu", wb, av), "hsel"], writes=[("bank", 6)])
                P.op("act", lambda e: e.copy(out=uhs[:].rearrange("p a b c -> p (a b c)"), in_=ps[6][:, 0:64]), reads=[("bank", 6)], writes=["uhs"])
                for tg in range(4):
                    ts = slice(tg * 512, (tg + 1) * 512)
                    for av in range(2):
                        b, bt = ps[(tg * 2 + av) % 4], ("bank", (tg * 2 + av) % 4)
                        for kt in range(8):
                            P.op("pe", lambda e, b=b, av=av, kt=kt, ts=ts: e.matmul(b[:], lhsT=wu[wb][:, kt, av, :], rhs=h2T[:, kt, ts], start=(kt == 0), stop=(kt == 7)),
                                 reads=[("wu", wb, av)], writes=[bt])
                        u_ = ue[av]
                        P.op("act", lambda e, b=b, u_=u_: e.copy(out=u_[:, :, 2:130], in_=b[:].rearrange("p (s r) -> p s r", s=4)), reads=[bt], writes=[("ue", av)])
                        P.op("pool", lambda e, u_=u_, av=av, tg=tg: e.tensor_copy(out=u_[:, :, 0:2], in_=uhs[:, av, 4 * tg:4 * tg + 4, :]), reads=["uhs"], writes=[("ue", av)])
                        ti = av * 22 + ct
                        a_ = acc[av]
                        P.op("dve", lambda e, u_=u_, a_=a_, ti=ti: e.tensor_scalar(out=a_[:], in0=u_[:, :, 0:128], scalar1=cw[:, 0, ti:ti + 1], scalar2=cb[:, ti:ti + 1],
                                                                                 op0=ALU.mult, op1=ALU.add), reads=[("ue", av), "cw", "cb"], writes=[("acc", av)])
                        for kk in (1, 2):
                            P.op("dve", lambda e, u_=u_, a_=a_, ti=ti, kk=kk: e.scalar_tensor_tensor(out=a_[:], in0=u_[:, :, kk:kk + 128], scalar=cw[:, kk, ti:ti + 1], in1=a_[:],
                                                                                                   op0=ALU.mult, op1=ALU.add), reads=[("ue", av), "cw", ("acc", av)], writes=[("acc", av)])
                    P.op("act", lambda e: e.activation(out=sa[:], in_=acc[0][:].rearrange("p s r -> p (s r)"), func=AF.Silu), reads=[("acc", 0)], writes=["sa"])
                    P.op("pool", lambda e, ts=ts: e.tensor_tensor(out=gT[:, ct, ts], in0=sa[:], in1=acc[1][:].rearrange("p s r -> p (s r)"), op=ALU.mult),
                         reads=["sa", ("acc", 1)], writes=[("gT", ct)])

            for ct in range(22):
                do_ct(ct)
            P.emit()
        with ExitStack() as st:
            sb = lambda name, shape, dt: st.enter_context(nc.sbuf_tensor("sb_" + name, shape, dt))
            wd = sb("wd", [128, 22, D], BF16)
            gfT = sb("gfT", [128, 8], F32)
            x1g = sb("x1g", [128, 8, 512], F32)
            x2T = sb("x2T", [128, 8, 512], F32)
            oT = x1g
            sq = sb("sq5", [128, 2, 512], BF16)
            rstd = sb("rstd5", [128, 512], F32)
            ntmp = sb("ntmp5", [128, 1, 512], F32)
            otok = sb("otok", [128, 1, D], F32)
            ld(k, gfT[:], I["gfT"], "gfT", "gfT")
            wdv = I["w_down"].rearrange("p (t o) -> p t o", t=22)
            for q4 in range(4):
                lo, hi = [0, 6, 12, 17][q4], [6, 12, 17, 22][q4]
                P.dma("pool", wd[:, lo:hi, :], wdv[:, lo:hi, :], writes=[("wd", q4)], key=("wd", q4))
            wd_reads = [("wd", q4) for q4 in range(4)]

            def do_tg(tg):
                ts = slice(tg * 512, (tg + 1) * 512)
                P.dma("sp", x1g[:], k.x1d[:, :, ts], writes=["x1g"] + [("x2", "dst", ft) for ft in range(8)], key="x1g")
                for ot in range(8):
                    b, bt = ps[ot % 4], ("bank", ot % 4)
                    for ct in range(22):
                        P.op("pe", lambda e, b=b, ot=ot, ct=ct: e.matmul(b[:], lhsT=wd[:, ct, ot * 128:(ot + 1) * 128], rhs=gT[:, ct, ts], start=(ct == 0), stop=(ct == 21)),
                             reads=wd_reads, writes=[bt])
                    P.op("dve", lambda e, b=b, ot=ot: e.scalar_tensor_tensor(out=x2T[:, ot, :], in0=b[:], scalar=k.modT[:, 40 + ot:41 + ot], in1=x1g[:, ot, :],
                                                                            op0=ALU.mult, op1=ALU.add), reads=[bt, "x1g"], writes=[("x2", "src", ot)])
                rmsnorm_T(k, ("x2",), [x2T[:, ft, :] for ft in range(8)], [oT[:, ft, :] for ft in range(8)], gfT, None, sq, rstd, ntmp, ps[7])
                for s_ in range(4):
                    for hf in range(2):
                        b, bt = ps[4 + hf], ("bank", 4 + hf)
                        for f4 in range(4):
                            ft = hf * 4 + f4
                            P.op("pe", lambda e, b=b, s_=s_, ft=ft, f4=f4: e.transpose(out=b[:, f4 * 128:(f4 + 1) * 128], in_=oT[:, ft, s_ * 128:(s_ + 1) * 128], identity=k.identF[:]),
                                 reads=[("x2", "dst", ft), "identF"], writes=[bt])
                        P.op("act", lambda e, b=b, s_=s_, hf=hf: e.copy(out=otok[:, 0, hf * 512:(hf + 1) * 512], in_=b[:]), reads=[bt], writes=["otok"])
                    P.dma("sp", k.out[tg * 512 + s_ * 128:tg * 512 + (s_ + 1) * 128, :], otok[:, 0, :], reads=["otok"], writes=[("out", tg, s_)], key="out")

            for tg in range(4):
                do_tg(tg)
            P.emit()


def kernel(**inputs):
    maps = host_prep(inputs)
    nc = build(stop_after=5)
    res = run_bass_kernel_spmd(nc, maps, core_ids=list(range(NCORE)))
    out = np.zeros((NSEG, NCORE, 128, D), np.float32)
    for c in range(NCORE):
        out[:, c] = np.asarray(res.results[c]["out"]).reshape(NSEG, 128, D)
    return out.reshape(1, NSEG * NCORE * 128, D)
```
